# Optimizing a Trainium2 kernel written in Bass

```python
import math
import jax, jax.numpy as jnp
from jax import lax
import numpy as np

D_MODEL = 1024
BATCH = 8
SEQ = 8192
DEPTH = 2

CHUNK = 64
D_MIX = D_MODEL
POOL_WIDTH = D_MIX // 4
DIFF_WIDTH = D_MIX // 2
GLA_WIDTH = D_MIX // 4
POOL_WINDOWS = (2, 4, 8, 16)
POOL_GROUPS = len(POOL_WINDOWS)
POOL_GDIM = POOL_WIDTH // POOL_GROUPS
DIFF_HEADS = 4
DIFF_VDIM = DIFF_WIDTH // DIFF_HEADS
DIFF_QKDIM = DIFF_VDIM // 2
GLA_HEADS = 4
GLA_VDIM = GLA_WIDTH // GLA_HEADS
GLA_KDIM = GLA_VDIM // 2
GLA_GATE_RANK = 16
GLA_GATE_TAU = 16.0
D_FF = 4 * D_MODEL
Q_BLOCK = 128
EPS = 1e-6

SIZES = (
    POOL_WIDTH,
    DIFF_HEADS * 2 * DIFF_QKDIM,
    DIFF_HEADS * 2 * DIFF_QKDIM,
    DIFF_HEADS * DIFF_VDIM,
    GLA_HEADS * GLA_KDIM,
    GLA_HEADS * GLA_KDIM,
    GLA_WIDTH,
    GLA_WIDTH,
    GLA_GATE_RANK,
)
D_IN_PROJ = sum(SIZES)
SPLIT_POINTS = tuple(sum(SIZES[:i + 1]) for i in range(len(SIZES) - 1))

kernel_name = "hybrid_pool_diffattn_gla_trunk"


def rms_norm(x, g):
    xf = x.astype(jnp.float32)
    y = xf * lax.rsqrt(jnp.mean(xf * xf, axis=-1, keepdims=True) + EPS)
    return (y * g.astype(jnp.float32)).astype(x.dtype)


def pool_mixer(u, w_pool, pool_scale):
    B, S, _ = u.shape
    uf = u.astype(jnp.float32).reshape(B, S, POOL_GROUPS, POOL_GDIM)
    csum = jnp.cumsum(uf, axis=1)
    t = jnp.arange(S)
    means = []
    for g, win in enumerate(POOL_WINDOWS):
        c = csum[:, :, g]
        c_prev = jnp.pad(c, ((0, 0), (win, 0), (0, 0)))[:, :S]
        cnt = jnp.minimum(t + 1, win).astype(jnp.float32)[None, :, None]
        means.append((c - c_prev) / cnt)
    pooled = jnp.stack(means, axis=2) - uf
    mixed = jnp.einsum('bsgc,gcd->bsgd', pooled, w_pool.astype(jnp.float32))
    return (mixed.reshape(B, S, POOL_WIDTH) * pool_scale.astype(jnp.float32)).astype(u.dtype)


def diff_attention(q, k, v, lq1, lk1, lq2, lk2, norm_g, lambda_init):
    B, S = q.shape[:2]
    nqb = S // Q_BLOCK
    f32 = jnp.float32
    lam = (jnp.exp(jnp.sum(lq1.astype(f32) * lk1.astype(f32)))
           - jnp.exp(jnp.sum(lq2.astype(f32) * lk2.astype(f32))) + lambda_init)
    scale = DIFF_QKDIM ** -0.5
    k_chunk = jnp.arange(S) // CHUNK
    qb = q.reshape(B, nqb, Q_BLOCK, DIFF_HEADS, 2, DIFF_QKDIM).transpose(1, 0, 2, 3, 4, 5)

    def one_block(args):
        q_blk, blk = args
        s = jnp.einsum('bqhmd,bkhmd->bhmqk', q_blk, k).astype(f32) * scale
        q_chunk = (blk * Q_BLOCK + jnp.arange(Q_BLOCK)) // CHUNK
        mask = k_chunk[None, :] <= q_chunk[:, None]
        s = jnp.where(mask, s, -jnp.inf)
        p = jax.nn.softmax(s, axis=-1)
        a = p[:, :, 0] - lam * p[:, :, 1]
        return jnp.einsum('bhqk,bkhd->bqhd', a.astype(v.dtype), v)

    o = lax.map(one_block, (qb, jnp.arange(nqb)))
    o = o.transpose(1, 0, 2, 3, 4).reshape(B, S, DIFF_HEADS, DIFF_VDIM)
    o = rms_norm(o, norm_g.reshape(DIFF_HEADS, DIFF_VDIM)) * (1.0 - lambda_init)
    return o.reshape(B, S, DIFF_WIDTH)


def gla_mixer(q, k, v, r, g_lr, w_gate2, b_gate, norm_g):
    B, S = q.shape[:2]
    nc = S // CHUNK
    f32 = jnp.float32
    log_a = jax.nn.log_sigmoid((g_lr @ w_gate2 + b_gate).astype(f32)) / GLA_GATE_TAU
    log_a = log_a.reshape(B, S, GLA_HEADS, GLA_KDIM)

    def to_chunks(t):
        return t.astype(f32).reshape(B, nc, CHUNK, GLA_HEADS, t.shape[-1]).transpose(1, 0, 3, 2, 4)

    qc = to_chunks(q) * (GLA_KDIM ** -0.5)
    kc, vc, lc = to_chunks(k), to_chunks(v), to_chunks(log_a)
    bcum = jnp.cumsum(lc, axis=3)
    b_last = bcum[:, :, :, -1, :]
    q_dec = qc * jnp.exp(bcum)
    k_inv = kc * jnp.exp(-bcum)
    k_end = kc * jnp.exp(b_last[:, :, :, None, :] - bcum)
    causal = jnp.tril(jnp.ones((CHUNK, CHUNK), dtype=bool))
    att = jnp.where(causal, jnp.einsum('nbhid,nbhjd->nbhij', q_dec, k_inv), 0.0)
    o_intra = jnp.einsum('nbhij,nbhje->nbhie', att, vc)

    def step(state, xs):
        q_d, k_e, v_c, decay = xs
        o_inter = jnp.einsum('bhid,bhde->bhie', q_d, state)
        state = state * jnp.exp(decay)[..., None] + jnp.einsum('bhjd,bhje->bhde', k_e, v_c)
        return state, o_inter

    state0 = jnp.zeros((B, GLA_HEADS, GLA_KDIM, GLA_VDIM), f32)
    _, o_inter = lax.scan(step, state0, (q_dec, k_end, vc, b_last))
    o = (o_intra + o_inter).transpose(1, 0, 3, 2, 4).reshape(B, S, GLA_HEADS, GLA_VDIM)
    o = rms_norm(o, norm_g.reshape(GLA_HEADS, GLA_VDIM)).reshape(B, S, GLA_WIDTH)
    return (o * jax.nn.silu(r.astype(f32))).astype(r.dtype)


def setup_inputs(seed: int = 0) -> dict:
    key = jax.random.key(seed)
    ks = jax.random.split(key, 24)
    nrm = jax.random.normal
    f32 = jnp.float32
    L = DEPTH
    return {
        "x": nrm(ks[0], (BATCH, SEQ, D_MODEL), f32),
        "norm1_g": 1.0 + 0.05 * nrm(ks[1], (L, D_MODEL), f32),
        "w_in": nrm(ks[2], (L, D_MODEL, D_IN_PROJ), f32) * D_MODEL ** -0.5,
        "pool_w": nrm(ks[3], (L, POOL_GROUPS, POOL_GDIM, POOL_GDIM), f32) * POOL_GDIM ** -0.5,
        "pool_scale": 1.0 + 0.1 * nrm(ks[4], (L, POOL_WIDTH), f32),
        "diff_lq1": 0.1 * nrm(ks[5], (L, DIFF_QKDIM), f32),
        "diff_lk1": 0.1 * nrm(ks[6], (L, DIFF_QKDIM), f32),
        "diff_lq2": 0.1 * nrm(ks[7], (L, DIFF_QKDIM), f32),
        "diff_lk2": 0.1 * nrm(ks[8], (L, DIFF_QKDIM), f32),
        "diff_norm_g": 1.0 + 0.05 * nrm(ks[9], (L, DIFF_WIDTH), f32),
        "gla_w_gate2": nrm(ks[10], (L, GLA_GATE_RANK, GLA_HEADS * GLA_KDIM), f32) * GLA_GATE_RANK ** -0.5,
        "gla_b_gate": 0.1 * nrm(ks[11], (L, GLA_HEADS * GLA_KDIM), f32),
        "gla_norm_g": 1.0 + 0.05 * nrm(ks[12], (L, GLA_WIDTH), f32),
        "w_out": nrm(ks[13], (L, D_MIX, D_MODEL), f32) * D_MIX ** -0.5,
        "norm2_g": 1.0 + 0.05 * nrm(ks[14], (L, D_MODEL), f32),
        "w_mlp1": nrm(ks[15], (L, D_MODEL, D_FF), f32) * D_MODEL ** -0.5,
        "w_mlp2": nrm(ks[16], (L, D_FF, D_MODEL), f32) * D_FF ** -0.5,
        "final_norm_g": 1.0 + 0.05 * nrm(ks[17], (D_MODEL,), f32),
    }


def reference(x, norm1_g, w_in, pool_w, pool_scale, diff_lq1, diff_lk1, diff_lq2, diff_lk2,
              diff_norm_g, gla_w_gate2, gla_b_gate, gla_norm_g, w_out, norm2_g, w_mlp1,
              w_mlp2, final_norm_g):
    B, S, _ = x.shape
    h = x
    for l in range(DEPTH):
        u = rms_norm(h, norm1_g[l])
        proj = u @ w_in[l]
        p_in, dq, dk, dv, gq, gk, gv, gr, gg = jnp.split(proj, SPLIT_POINTS, axis=-1)
        y_pool = pool_mixer(p_in, pool_w[l], pool_scale[l])
        lambda_init = 0.8 - 0.6 * math.exp(-0.3 * l)
        y_diff = diff_attention(
            dq.reshape(B, S, DIFF_HEADS, 2, DIFF_QKDIM),
            dk.reshape(B, S, DIFF_HEADS, 2, DIFF_QKDIM),
            dv.reshape(B, S, DIFF_HEADS, DIFF_VDIM),
            diff_lq1[l], diff_lk1[l], diff_lq2[l], diff_lk2[l], diff_norm_g[l], lambda_init)
        y_gla = gla_mixer(
            gq.reshape(B, S, GLA_HEADS, GLA_KDIM),
            gk.reshape(B, S, GLA_HEADS, GLA_KDIM),
            gv.reshape(B, S, GLA_HEADS, GLA_VDIM),
            gr, gg, gla_w_gate2[l], gla_b_gate[l], gla_norm_g[l])
        mix = jnp.concatenate([y_pool, y_diff.astype(y_pool.dtype), y_gla.astype(y_pool.dtype)], axis=-1)
        h = h + (mix @ w_out[l]).astype(h.dtype)
        z = rms_norm(h, norm2_g[l])
        h = h + jnp.square(jax.nn.relu(z @ w_mlp1[l])) @ w_mlp2[l]
    return rms_norm(h, final_norm_g)
```

```python
import math
import numpy as np
import concourse.bass as bass
import concourse.mybir as mybir
from concourse.bass_utils import run_bass_kernel_spmd

F32 = mybir.dt.float32
BF16 = mybir.dt.bfloat16
ALU = mybir.AluOpType
AF = mybir.ActivationFunctionType
AX = mybir.AxisListType

L = 2
D = 1024
DIN = 2576
DFF = 4096
EPS = 1e-6
SEM_ROT = 24000
import os
SKIP = set(os.environ.get('K_SKIP', '').split(','))
ATTACH = os.environ.get('K_ATTACH', '1') == '1'


class Buf:
    __slots__ = ("name", "w", "r", "dsem", "dcnt", "excl")

    def __init__(self, name):
        self.name = name
        self.excl = False
        self.w = []
        self.r = []
        self.dsem = None
        self.dcnt = 0


class Eng:
    def __init__(self, name):
        self.name = name
        self.sem = None
        self.cnt = 0
        self.seen = {}
        self.recs = []


def _merge(evts):
    d = {}
    for (s, v) in evts:
        k = id(s)
        if k not in d or d[k][1] < v:
            d[k] = (s, v)
    return list(d.values())


class Sched:
    def __init__(self, nc):
        self.nc = nc
        self.engs = {n: Eng(n) for n in ("pe", "act", "dve", "pool", "sp")}
        self.nsem = 0
        self.ninst = 0
        self.dbufs = []
        self.cache = {}
        self.engsems = set()

    def new_sem(self, name):
        self.nsem += 1
        sm = self.nc.alloc_semaphore(name=f"{name}_{self.nsem}")
        if name.startswith("p_"):
            self.engsems.add(id(sm))
        return sm

    def buf(self, name):
        if name not in self.cache:
            self.cache[name] = Buf(name)
        return self.cache[name]

    def _deps(self, reads, writes):
        ev = []
        for b in reads:
            ev.extend(b.w)
        for b in writes:
            ev.extend(b.w)
            ev.extend(b.r)
        return _merge(ev)

    def _need(self, eng, evts):
        waits = []
        for (s, v) in evts:
            k = id(s)
            if k in eng.seen and eng.seen[k][1] >= v:
                continue
            eng.seen[k] = (s, v)
            waits.append((s, v))
        return waits

    def _commit(self, ev, reads, writes):
        for b in writes:
            b.w = [ev]
            b.r = []
        for b in reads:
            if b in writes:
                continue
            b.r = [e for e in b.r if e[0] is not ev[0]] + [ev]

    def _eng_ev(self, eng):
        if eng.sem is None or eng.cnt >= SEM_ROT:
            eng.sem = self.new_sem("p_" + eng.name)
            eng.cnt = 0
        eng.cnt += 1
        return (eng.sem, eng.cnt)

    def op(self, engname, fn, reads=(), writes=()):
        return self.group(engname, [fn], reads, writes)

    def group(self, engname, fns, reads=(), writes=(), attach=None):
        eng = self.engs[engname]
        if attach is None:
            attach = ATTACH
        xr = [b for b in reads if b.excl]
        if xr:
            reads = [b for b in reads if not b.excl]
            writes = list(writes) + [b for b in xr if b not in writes]
        rdeps = self._deps(reads, ())
        waits = self._need(eng, self._deps(reads, writes))
        ev = self._eng_ev(eng)
        aw = None
        if attach and ATTACH:
            rd = {id(s_): v_ for (s_, v_) in rdeps}
            for w_ in reversed(waits):
                if id(w_[0]) not in self.engsems:
                    continue
                if engname == "pe" and id(w_[0]) in rd and rd[id(w_[0])] >= w_[1]:
                    continue
                aw = w_
                break
            if aw is not None:
                waits = [w_ for w_ in waits if w_ is not aw]
        eng.recs.append((waits, list(fns), (ev[0], 1, False), aw))
        self._commit(ev, reads, writes)
        self.ninst += len(fns)
        return ev

    def dma(self, qname, fns, reads=(), writes=(), sembuf=None):
        if not isinstance(fns, (list, tuple)):
            fns = [fns]
        eng = self.engs[qname]
        sb = sembuf
        if sb.dsem is None or sb.dcnt + 16 * len(fns) >= SEM_ROT:
            sb.dsem = self.new_sem("d_" + sb.name)
            sb.dcnt = 0
            self.dbufs.append(sb)
        waits = self._need(eng, self._deps(reads, writes))
        sb.dcnt += 16 * len(fns)
        ev = (sb.dsem, sb.dcnt)
        eng.recs.append((waits, list(fns), (sb.dsem, 16, True)))
        self._commit(ev, reads, writes)
        self.ninst += len(fns)
        return ev

    def barrier(self):
        ev = []
        for e in self.engs.values():
            if e.sem is not None:
                ev.append((e.sem, e.cnt))
        seen_ids = set()
        for b in self.dbufs:
            if b.dsem is not None and id(b.dsem) not in seen_ids:
                seen_ids.add(id(b.dsem))
                ev.append((b.dsem, b.dcnt))
        ev = _merge(ev)
        for e in self.engs.values():
            waits = self._need(e, ev)
            if waits:
                e.recs.append((waits, [], None))

    def finish(self):
        self.barrier()
        nc = self.nc

        def replay(eng):
            def run(e):
                for rec in eng.recs:
                    waits, fns, inc = rec[0], rec[1], rec[2]
                    aw = rec[3] if len(rec) > 3 else None
                    for (s, v) in waits:
                        e.wait_ge(s, v)
                    last = None
                    for f in fns:
                        last = f(e)
                        if aw is not None:
                            last._wait_ge(aw[0], aw[1])
                            aw = None
                        if inc is not None and inc[2]:
                            last.then_inc(inc[0], inc[1])
                    if inc is not None and not inc[2] and last is not None:
                        last.then_inc(inc[0], inc[1])
            return run

        with nc.Block() as block:
            block.tensor(replay(self.engs["pe"]))
            block.scalar(replay(self.engs["act"]))
            block.vector(replay(self.engs["dve"]))
            block.gpsimd(replay(self.engs["pool"]))
            block.sync(replay(self.engs["sp"]))


C_ID = 0
C_BD = 128
C_HM = 384
C_CORR = 388
C_G1 = 420
C_G2 = C_G1 + L * 8
C_GF = C_G2 + L * 8
C_PSC = C_GF + 8
C_GD = C_PSC + L * 2
C_BG = C_GD + L * 4
C_GGLA = C_BG + L
C_LQ = C_GGLA + L * 256
NC32 = C_LQ + L * 256
K_AM = 0
K_GM = 2048
K_IDB = 2560
NC16 = 2688

POOL_WINS = (2, 4, 8, 16)


def _const_tables():
    c32 = np.zeros((128, NC32), np.float32)
    c32[:, C_ID:C_ID + 128] = np.eye(128, dtype=np.float32)
    p = np.arange(128)
    c = np.arange(256)
    c32[:, C_BD:C_BD + 256] = (p[:, None] // 32 == c[None, :] // 64).astype(np.float32)
    c32[:, C_HM:C_HM + 4] = (p[:, None] // 32 == np.arange(4)[None, :]).astype(np.float32)
    corr = np.zeros((128, 2, 16), np.float32)
    for j in range(2):
        for half in range(2):
            w = POOL_WINS[2 * j + half]
            i = np.arange(16)
            corr[half * 64:(half + 1) * 64, j, :] = w / np.minimum(i + 1, w)
    c32[:, C_CORR:C_CORR + 32] = corr.reshape(128, 32)
    c16 = np.zeros((128, NC16), np.float32)
    jj = np.arange(128)
    ii = np.arange(512)
    for r in range(4):
        c16[:, K_AM + r * 512:K_AM + (r + 1) * 512] = (
            (2 * r + jj[:, None] // 64) <= (ii[None, :] // 64)).astype(np.float32)
    gm = (jj[:, None] <= np.arange(128)[None, :]).astype(np.float32)
    c16[:, K_GM:K_GM + 512] = np.tile(gm, (1, 4))
    c16[:, K_IDB:K_IDB + 128] = np.eye(128, dtype=np.float32)
    return c32, c16


def _fill_params(c32, inp):
    def cols(v, n):
        return np.ascontiguousarray(np.asarray(v, np.float32).reshape(n, 128).T)
    for l in range(L):
        c32[:, C_G1 + l * 8:C_G1 + (l + 1) * 8] = cols(inp["norm1_g"][l], 8)
        c32[:, C_G2 + l * 8:C_G2 + (l + 1) * 8] = cols(inp["norm2_g"][l], 8)
        c32[:, C_PSC + l * 2:C_PSC + (l + 1) * 2] = cols(inp["pool_scale"][l], 2)
        c32[:, C_GD + l * 4:C_GD + (l + 1) * 4] = cols(inp["diff_norm_g"][l], 4)
        c32[:, C_BG + l:C_BG + l + 1] = cols(inp["gla_b_gate"][l], 1)
        c32[:, C_GGLA + l * 256:C_GGLA + (l + 1) * 256] = np.asarray(inp["gla_norm_g"][l], np.float32)[None, :]
        for i, nm in enumerate(("diff_lq1", "diff_lk1", "diff_lq2", "diff_lk2")):
            c32[:, C_LQ + l * 256 + i * 64:C_LQ + l * 256 + (i + 1) * 64] = np.asarray(inp[nm][l], np.float32)[None, :]
    c32[:, C_GF:C_GF + 8] = cols(inp["final_norm_g"], 8)
    return c32


def build_nc(S_LEN, debug=False, n_layers=L, stop_after=None):
    NT = S_LEN // 512
    NKT = S_LEN // 128
    nc = bass.Bass("TRN2", target_bir_lowering=False)
    S = Sched(nc)

    def din(name, shape, dt=F32):
        return nc.dram_tensor(name, list(shape), dt, kind="ExternalInput").ap()

    x_d = din("x", [S_LEN, D])
    win_d = din("w_in", [L, D, DIN])
    wout_d = din("w_out", [L, D, D])
    w1_d = din("w_mlp1", [L, D, DFF])
    w2_d = din("w_mlp2", [L, DFF, D])
    wg2_d = din("wg2", [16, L * 128])
    poolw_d = din("pool_w", [L, 4, 64, 64])
    c32_d = din("c32", [128, NC32])
    c16_d = din("c16", [128, NC16])
    out_d = nc.dram_tensor("out", [S_LEN, D], F32, kind="ExternalOutput").ap()
    skind = "ExternalOutput" if debug else "Internal"

    def dscr(name, shape, dt):
        return nc.dram_tensor(name, list(shape), dt, kind=skind).ap()

    hT_d = dscr("hT", [D, S_LEN], F32)
    qT_d = dscr("qT", [8, 128, S_LEN], BF16)
    kT_d = dscr("kT", [4, 128, S_LEN], BF16)
    v4_d = dscr("v4", [4, 128, NKT, 128], BF16)
    mixT_d = dscr("mixT", [D, S_LEN], BF16)
    zT_d = dscr("zT", [D, S_LEN], BF16)

    def sbt(name, cols, dt=F32):
        return nc.sbuf_tensor(name, [128, cols], dt).__enter__()

    WA = sbt("WA", 16384)
    WB = sbt("WB", 16384)
    SCR = sbt("SCR", 16384)
    c32 = sbt("c32s", NC32)
    c16 = sbt("c16s", NC16, BF16)
    wg2 = sbt("wg2s", L * 128)
    poolW = sbt("poolW", L * 256, BF16)
    misc = sbt("misc", 64)
    ones16 = sbt("ones16", 128, BF16)
    onesD = sbt("onesD", 128, BF16)
    onesV = sbt("onesV", 128, BF16)
    ones32 = sbt("ones32", 128)
    PSALL = nc.psum_tensor("psall", [128, 4096], F32).__enter__()
    PS = [PSALL[:, i * 512:(i + 1) * 512] for i in range(8)]
    PSB = [S.buf(f"ps{i}") for i in range(8)]
    for b_ in PSB:
        b_.excl = True

    def view(t, off, shape, dt=F32):
        n = 1
        for s_ in shape:
            n *= s_
        nbytes = n * (4 if dt == F32 else 2)
        assert off % 4 == 0 and nbytes % 4 == 0 and off + nbytes <= 65536, (off, nbytes)
        ap = t[:, off // 4:(off + nbytes) // 4]
        if dt != F32:
            ap = ap.bitcast(dt)
        if len(shape) == 2:
            ap = ap.rearrange("p (a b) -> p a b", a=shape[0])
        elif len(shape) == 3:
            ap = ap.rearrange("p (a b c) -> p a b c", a=shape[0], b=shape[1])
        return ap

    def act(out, in_, func, reads, writes, **kw):
        S.op("act", lambda e: e.activation(out, in_, func, **kw), reads, writes)

    def tt(out, a, b, op, reads, writes, eng="dve"):
        S.op(eng, lambda e: e.tensor_tensor(out, a, b, op), reads, writes)

    def stt(out, in0, scalar, in1, op0, op1, reads, writes, eng="dve"):
        S.op(eng, lambda e: e.scalar_tensor_tensor(out, in0, scalar, in1, op0, op1), reads, writes)

    def ts(out, in0, s1, s2, op0, op1, reads, writes, eng="dve"):
        if op1 is None:
            S.op(eng, lambda e: e.tensor_scalar(out, in0, s1, None, op0), reads, writes)
        else:
            S.op(eng, lambda e: e.tensor_scalar(out, in0, s1, s2, op0, op1), reads, writes)

    def cp(out, in_, reads, writes, eng="dve"):
        if eng == "act":
            S.op("act", lambda e: e.activation(out, in_, AF.Copy), reads, writes)
        else:
            S.op(eng, lambda e: e.tensor_copy(out, in_), reads, writes)

    def mm(out, pairs, reads, writes, start=True, attach=False):
        n = len(pairs)
        fns = []
        for i, (lt, rh) in enumerate(pairs):
            fns.append(lambda e, lt=lt, rh=rh, i=i: e.matmul(out, lt, rh, start=(start and i == 0), stop=(i == n - 1)))
        S.group("pe", fns, reads, writes, attach=attach)

    def rsqrt_inplace(t_ap, buf, scale=1.0):
        act(t_ap, t_ap, AF.Ln, [buf], [buf], bias=EPS, scale=scale)
        act(t_ap, t_ap, AF.Exp, [buf], [buf], scale=-0.5)

    b_c32, b_c16, b_wg2, b_poolW, b_misc, b_ones = (S.buf(n) for n in ("c32", "c16", "wg2", "poolW", "misc", "ones"))
    S.dma("sp", lambda e: e.dma_start(out=c32[:], in_=c32_d), writes=[b_c32], sembuf=b_c32)
    S.op("dve", lambda e: e.memset(wg2[:], 0.0), writes=[b_wg2])
    S.dma("sp", lambda e: e.dma_start(out=wg2[112:128, :], in_=wg2_d), writes=[b_wg2], sembuf=b_wg2)
    S.dma("pool", lambda e: e.dma_start(out=c16[:], in_=c16_d), writes=[b_c16], sembuf=b_c16)
    S.op("dve", lambda e: e.memset(poolW[:], 0.0), writes=[b_poolW])
    pw_fns = []
    for l in range(L):
        for g in range(4):
            j, half = g // 2, g % 2
            pw_fns.append(lambda e, l=l, g=g, j=j, half=half: e.dma_start(
                out=poolW[half * 64:(half + 1) * 64, l * 256 + j * 128 + half * 64: l * 256 + j * 128 + half * 64 + 64],
                in_=poolw_d[l, g]))
    S.dma("pool", pw_fns, writes=[b_poolW], sembuf=b_poolW)
    S.op("dve", lambda e: e.memset(ones16[:], 1.0), writes=[b_ones])
    S.op("dve", lambda e: e.memset(onesD[:], 1.0 / 1024.0), writes=[b_ones])
    S.op("dve", lambda e: e.memset(onesV[:], 1.0 / 128.0), writes=[b_ones])
    S.op("dve", lambda e: e.memset(ones32[:], 1.0), writes=[b_ones])
    ident32 = c32[:, C_ID:C_ID + 128]
    identb = c16[:, K_IDB:K_IDB + 128]
    M_NLAM, M_NBG, M_GD = 0, 4, 8
    for l in range(L):
        lam_init = 0.8 - 0.6 * math.exp(-0.3 * l)
        lq = c32[:, C_LQ + l * 256:C_LQ + (l + 1) * 256]
        tmp = misc[:, 32:34]
        prod = view(SCR, 0, [256])
        b_tmp = S.buf("lamtmp")
        tt(prod[:, 0:64], lq[:, 0:64], lq[:, 64:128], ALU.mult, [b_c32], [b_tmp])
        tt(prod[:, 64:128], lq[:, 128:192], lq[:, 192:256], ALU.mult, [b_c32], [b_tmp])
        S.op("dve", lambda e, prod=prod, tmp=tmp: e.tensor_reduce(
            tmp, prod[:, 0:128].rearrange("p (a b) -> p a b", a=2), AX.X, ALU.add), [b_tmp], [b_misc])
        act(tmp, tmp, AF.Exp, [b_misc], [b_misc])
        stt(misc[:, M_NLAM + l:M_NLAM + l + 1], tmp[:, 1:2], -lam_init, tmp[:, 0:1], ALU.add, ALU.subtract, [b_misc], [b_misc])
        ts(misc[:, M_NBG + l:M_NBG + l + 1], c32[:, C_BG + l:C_BG + l + 1], -1.0, None, ALU.mult, None, [b_c32], [b_misc])
        ts(misc[:, M_GD + 4 * l:M_GD + 4 * l + 4], c32[:, C_GD + 4 * l:C_GD + 4 * l + 4], 1.0 - lam_init, None, ALU.mult, None,
           [b_c32], [b_misc])

    b_win, b_wout = S.buf("win"), S.buf("wout")
    b_w1 = [S.buf("w1a"), S.buf("w1b")]
    b_w2 = [S.buf("w2a"), S.buf("w2b")]
    WinV = view(WA, 0, [8, DIN], BF16)
    WoutV = view(WB, 0, [8, D], BF16)
    W1V = [view(WA, 0, [8, 2048], BF16), view(WB, 0, [8, 2048], BF16)]
    W2V = [view(WA, 32768, [16, D], BF16), view(WB, 32768, [16, D], BF16)]

    def load_win(l):
        S.dma("pool", [lambda e, kc=kc: e.dma_start(out=WinV[:, kc, :], in_=win_d[l, kc * 128:(kc + 1) * 128, :])
                       for kc in range(8)], writes=[b_win], sembuf=b_win)

    def load_wout(l):
        S.dma("pool", [lambda e, kc=kc: e.dma_start(out=WoutV[:, kc, :], in_=wout_d[l, kc * 128:(kc + 1) * 128, :])
                       for kc in range(8)], writes=[b_wout], sembuf=b_wout)

    def load_w12(l, hf):
        S.dma("pool", [lambda e, kc=kc: e.dma_start(out=W1V[hf][:, kc, :],
                                                    in_=w1_d[l, kc * 128:(kc + 1) * 128, hf * 2048:(hf + 1) * 2048])
                       for kc in range(8)], writes=[b_w1[hf]], sembuf=b_w1[hf])
        S.dma("pool", [lambda e, f=f: e.dma_start(out=W2V[hf][:, f, :],
                                                  in_=w2_d[l, (hf * 16 + f) * 128:(hf * 16 + f + 1) * 128, :])
                       for f in range(16)], writes=[b_w2[hf]], sembuf=b_w2[hf])

    load_win(0)
    load_wout(0)

    def phase_A(l):
        g1 = c32[:, C_G1 + l * 8:C_G1 + (l + 1) * 8]
        hA = view(SCR, 0, [8, 512])
        uTs = [view(SCR, 16384, [8, 512], BF16), view(WA, 43008, [8, 512], BF16)]
        xs = view(SCR, 24576, [2, 1024])
        sq = view(SCR, 32768, [2, 512], BF16)
        rstd = view(SCR, 34816, [512])
        pE = view(SCR, 36864, [2, 528])
        pX = view(SCR, 41088, [2, 528])
        pY = view(SCR, 45312, [2, 528])
        pM = view(SCR, 49536, [2, 512])
        pL = view(SCR, 53632, [2, 512], BF16)
        mixp = view(SCR, 55680, [2, 512], BF16)
        mixg = view(SCR, 57728, [2, 512], BF16)
        gg32 = view(SCR, 59776, [512])
        S32 = view(SCR, 61824, [256])
        Sbf = view(SCR, 62848, [256], BF16)
        stmp = view(SCR, 63360, [256])
        ssq4 = view(SCR, 64384, [4])
        qst = view(WB, 16384, [8, 512], BF16)
        kst = view(WB, 24576, [4, 512], BF16)
        vst = view(WB, 28672, [4, 512], BF16)
        gq32 = view(WB, 32768, [512])
        gk32 = view(WB, 34816, [512])
        la = view(WB, 36864, [512])
        Bn = view(WB, 38912, [512])
        eq = view(WB, 40960, [512])
        ek = view(WB, 43008, [512])
        qd16 = view(WB, 45056, [512], BF16)
        ki16 = view(WB, 46080, [512], BF16)
        Qm = view(WB, 47104, [4, 4, 128], BF16)
        kitok = view(WB, 51200, [4, 128], BF16)
        gv16 = view(WB, 52224, [4, 256], BF16)
        R = view(WB, 54272, [4, 256])
        Eb = view(WB, 58368, [4, 256])
        attm = view(WB, 62464, [512], BF16)
        o32 = view(WB, 63488, [256])
        osq = view(WB, 64512, [256])
        t1 = view(WA, 41216, [256])
        y16 = view(WA, 42240, [256], BF16)

        b_hA = [S.buf(f"hA{k}") for k in range(8)]
        b_uTs = [[S.buf(f"uT{k}") for k in range(8)], [S.buf(f"uTb{k}") for k in range(8)]]
        DEEP = (l > 0)

        def upar(t):
            return (t % 2) if DEEP else 0
        b_xs = [S.buf("xs0"), S.buf("xs1")]
        b_sq = [S.buf("sq0"), S.buf("sq1")]
        b_rstd = S.buf("rstd")
        b_pE, b_pX, b_pY, b_pM, b_pL = (S.buf(n) for n in ("pE", "pX", "pY", "pM", "pL"))
        b_mixp, b_mixg = S.buf("mixp"), S.buf("mixg")
        b_gg, b_S32, b_Sbf, b_stmp, b_ssq4 = (S.buf(n) for n in ("gg", "S32", "Sbf", "stmp", "ssq4"))
        b_qst, b_kst, b_vst = S.buf("qst"), S.buf("kst"), S.buf("vst")
        b_gq, b_gk, b_la, b_Bn, b_eq, b_ek = (S.buf(n) for n in ("gq", "gk", "la", "Bn", "eq", "ek"))
        b_qd, b_ki, b_Qm, b_kitok, b_gv, b_R, b_Eb = (S.buf(n) for n in ("qd", "ki", "Qm", "kitok", "gv", "R", "Eb"))
        b_attm, b_o32, b_osq, b_t1, b_y16 = (S.buf(n) for n in ("attm", "o32", "osq", "t1", "y16"))
        b_hst = S.buf("hst")

        S.op("dve", lambda e: e.memset(qst, 0.0), writes=[b_qst])
        S.op("dve", lambda e: e.memset(pE[:, :, 0:16], 0.0), writes=[b_pE])
        S.op("dve", lambda e: e.memset(S32, 0.0), writes=[b_S32])
        S.op("dve", lambda e: e.memset(Sbf, 0.0), writes=[b_Sbf])
        projbanks = [3, 4] if l == 0 else [3, 4, 0, 1]
        pbi = [0]

        def nextbank():
            b = projbanks[pbi[0] % len(projbanks)]
            pbi[0] += 1
            return b

        def load_tile(t):
            if l == 0:
                return
            for kc in range(8):
                S.dma("sp", lambda e, kc=kc: e.dma_start(out=hA[:, kc, :], in_=hT_d[kc * 128:(kc + 1) * 128, t * 512:(t + 1) * 512]),
                      writes=[b_hA[kc]], sembuf=b_hA[kc])

        if l > 0:
            load_tile(0)
        xs4 = view(WA, 43008, [4, 1024])
        b_xs4 = [S.buf(f"xs4_{i}") for i in range(4)]

        def load_x(t):
            for s in range(4):
                S.dma("sp", lambda e, s=s, t=t: e.dma_start(out=xs4[:, s, :], in_=x_d[t * 512 + s * 128:t * 512 + (s + 1) * 128, :]),
                      writes=[b_xs4[s]], sembuf=b_xs4[s])

        cur = [0]

        def proj_fm(col0, M):
            bank = nextbank()
            uT = uTs[cur[0]]
            mm(PS[bank][0:M, :], [(WinV[:, kc, col0:col0 + M], uT[:, kc, :]) for kc in range(8)],
               b_uTs[cur[0]] + [b_win], [PSB[bank]])
            return bank

        def proj_tm(col0, s):
            bank = nextbank()
            uT = uTs[cur[0]]
            mm(PS[bank][:, :], [(uT[:, kc, s * 128:(s + 1) * 128], WinV[:, kc, col0:col0 + 512]) for kc in range(8)],
               b_uTs[cur[0]] + [b_win], [PSB[bank]])
            return bank

        def norm(t):
            c0 = t * 512
            uT = uTs[upar(t)]
            b_uT = b_uTs[upar(t)]
            if l == 0:
                for s in range(4):
                    for half in range(2):
                        bank = half
                        fns = []
                        for kk in range(4):
                            kc = half * 4 + kk
                            fns.append(lambda e, kc=kc, kk=kk, s=s, bank=bank: e.transpose(
                                PS[bank][:, kk * 128:(kk + 1) * 128], xs4[:, s, kc * 128:(kc + 1) * 128], ident32))
                        S.group("pe", fns, [b_xs4[s], b_c32], [PSB[bank]])
                        outv = hA[:, half * 4:(half + 1) * 4, s * 128:(s + 1) * 128]
                        inv = PS[bank][:, :].rearrange("p (a b) -> p a b", a=4)
                        wr = [b_hA[half * 4 + kk] for kk in range(4)]
                        cp(outv, inv, [PSB[bank]], wr, eng="act" if half == 0 else "dve")
                    yield
                if t + 1 < NT:
                    load_x(t + 1)
                S.dma("sp", [lambda e, kc=kc, c0=c0: e.dma_start(out=hT_d[kc * 128:(kc + 1) * 128, c0:c0 + 512], in_=hA[:, kc, :])
                             for kc in range(8)], reads=b_hA, sembuf=b_hst)
            for kc in range(8):
                act(sq[:, kc % 2, :], hA[:, kc, :], AF.Square, [b_hA[kc]], [b_sq[kc % 2]])
                S.group("pe", [lambda e, kc=kc: e.matmul(PS[2][:, :], onesD[:, :], sq[:, kc % 2, :], start=(kc == 0), stop=(kc == 7))],
                        [b_sq[kc % 2], b_ones], [PSB[2]])
                if kc % 2 == 1:
                    yield
            act(rstd, PS[2][:, :], AF.Ln, [PSB[2]], [b_rstd], bias=EPS)
            act(rstd, rstd, AF.Exp, [b_rstd], [b_rstd], scale=-0.5)
            for kc in range(8):
                stt(uT[:, kc, :], hA[:, kc, :], g1[:, kc:kc + 1], rstd, ALU.mult, ALU.mult,
                    [b_hA[kc], b_rstd, b_c32], [b_uT[kc]])
            if l > 0 and t + 1 < NT:
                load_tile(t + 1)
            yield

        def projs(t):
            c0 = t * 512
            for j in range(2):
                cur[0] = upar(t)
                bk = proj_fm(j * 128, 128)
                cp(pE[:, j, 16:528], PS[bk][:, :], [PSB[bk]], [b_pE], eng="act")
                yield
            for h in range(4):
                cur[0] = upar(t)
                bk = proj_fm(256 + h * 128, 128)
                act(qst[0:64, 2 * h, :], PS[bk][0:64, :], AF.Copy, [PSB[bk]], [b_qst], scale=0.125)
                act(qst[64:128, 2 * h + 1, :], PS[bk][64:128, :], AF.Copy, [PSB[bk]], [b_qst], scale=0.125)
                yield
            S.dma("sp", lambda e, c0=c0: e.dma_start(out=qT_d[:, :, c0:c0 + 512].rearrange("a p s -> p a s"), in_=qst),
                  reads=[b_qst], sembuf=b_qst)
            PM = "dve"
            tt(pX[:, :, 1:528], pE[:, :, 1:528], pE[:, :, 0:527], ALU.add, [b_pE], [b_pX], eng=PM)
            tt(pY[64:128, 0, 3:528], pX[64:128, 0, 3:528], pX[64:128, 0, 1:526], ALU.add, [b_pX], [b_pY], eng=PM)
            tt(pY[:, 1, 3:528], pX[:, 1, 3:528], pX[:, 1, 1:526], ALU.add, [b_pX], [b_pY], eng=PM)
            tt(pX[:, 1, 7:528], pY[:, 1, 7:528], pY[:, 1, 3:524], ALU.add, [b_pY], [b_pX], eng=PM)
            tt(pY[64:128, 1, 15:528], pX[64:128, 1, 15:528], pX[64:128, 1, 7:520], ALU.add, [b_pX], [b_pY], eng=PM)
            for h in range(4):
                cur[0] = upar(t)
                bk = proj_fm(768 + h * 128, 128)
                cp(kst[:, h, :], PS[bk][:, :], [PSB[bk]], [b_kst], eng="act")
                yield
            S.dma("sp", lambda e, c0=c0: e.dma_start(out=kT_d[:, :, c0:c0 + 512].rearrange("a p s -> p a s"), in_=kst),
                  reads=[b_kst], sembuf=b_kst)
            ts(pM[0:64, 0, :], pX[0:64, 0, 16:528], 0.5, None, ALU.mult, None, [b_pX], [b_pM], eng=PM)
            ts(pM[64:128, 0, :], pY[64:128, 0, 16:528], 0.25, None, ALU.mult, None, [b_pY], [b_pM], eng=PM)
            ts(pM[0:64, 1, :], pX[0:64, 1, 16:528], 0.125, None, ALU.mult, None, [b_pX], [b_pM], eng=PM)
            ts(pM[64:128, 1, :], pY[64:128, 1, 16:528], 0.0625, None, ALU.mult, None, [b_pY], [b_pM], eng=PM)
            if t == 0:
                corr = c32[:, C_CORR:C_CORR + 32].rearrange("p (a b) -> p a b", a=2)
                tt(pM[:, :, 0:16], pM[:, :, 0:16], corr, ALU.mult, [b_pM, b_c32], [b_pM], eng=PM)
            tt(pL, pM, pE[:, :, 16:528], ALU.subtract, [b_pM, b_pE], [b_pL], eng=PM)
            cp(pE[:, :, 0:16], pE[:, :, 512:528], [b_pE], [b_pE], eng=PM)
            for s in range(4):
                cur[0] = upar(t)
                bk = proj_tm(1280, s)
                cp(vst[:, s, :], PS[bk][:, :], [PSB[bk]], [b_vst], eng="act" if s % 2 == 0 else "dve")
                yield
            S.dma("sp", [lambda e, s=s, t=t: e.dma_start(out=v4_d[:, :, 4 * t + s, :].rearrange("h p d -> p h d"),
                                                    in_=vst[:, s, :].rearrange("p (h d) -> p h d", h=4))
                         for s in range(4)], reads=[b_vst], sembuf=b_vst)
            for j in range(2):
                bk = nextbank()
                mm(PS[bk][:, :], [(poolW[:, l * 256 + j * 128:l * 256 + (j + 1) * 128], pL[:, j, :])], [b_pL, b_poolW], [PSB[bk]])
                ts(mixp[:, j, :], PS[bk][:, :], c32[:, C_PSC + 2 * l + j:C_PSC + 2 * l + j + 1], None, ALU.mult, None,
                   [PSB[bk], b_c32], [b_mixp])
            S.dma("sp", lambda e, c0=c0: e.dma_start(out=mixT_d[0:256, c0:c0 + 512].rearrange("(j p) s -> p j s", p=128), in_=mixp),
                  reads=[b_mixp], sembuf=b_mixp)
            yield

        def stage1b(t):
            cur[0] = upar(t)
            bk = proj_fm(1792, 128)
            cp(gq32, PS[bk][:, :], [PSB[bk]], [b_gq], eng="act")
            bk = proj_fm(1920, 128)
            cp(gk32, PS[bk][:, :], [PSB[bk]], [b_gk], eng="dve")
            bk = proj_fm(2448, 128)
            cp(gg32, PS[bk][:, :], [PSB[bk]], [b_gg], eng="act")
            for s in range(4):
                bk = proj_tm(2048, s)
                cp(gv16[:, s, :], PS[bk][:, 0:256], [PSB[bk]], [b_gv], eng="dve")
                act(Eb[:, s, :], PS[bk][:, 256:512], AF.Exp, [PSB[bk]], [b_Eb], scale=-1.0)
                cp(R[:, s, :], PS[bk][:, 256:512], [PSB[bk]], [b_R], eng="act")

        def gla(t):
            c0 = t * 512
            mm(PS[5][:, :], [(wg2[:, l * 128:(l + 1) * 128], gg32)], [b_wg2, b_gg], [PSB[5]])
            act(la, PS[5][:, :], AF.Exp, [PSB[5], b_misc], [b_la], scale=-1.0, bias=misc[:, M_NBG + l:M_NBG + l + 1])
            act(la, la, AF.Ln, [b_la], [b_la], bias=1.0)
            yield
            for s in range(4):
                S.op("dve", lambda e, s=s: e.tensor_tensor_scan(Bn[:, s * 128:(s + 1) * 128], ones32[:, :], la[:, s * 128:(s + 1) * 128],
                                                                0.0, ALU.mult, ALU.add), [b_la, b_ones], [b_Bn])
            act(eq, Bn, AF.Exp, [b_Bn], [b_eq], scale=-1.0 / 16.0)
            act(ek, Bn, AF.Exp, [b_Bn], [b_ek], scale=1.0 / 16.0)
            stt(qd16, gq32, 32 ** -0.5, eq, ALU.mult, ALU.mult, [b_gq, b_eq], [b_qd])
            tt(ki16, gk32, ek, ALU.mult, [b_gk, b_ek], [b_ki])
            for h in range(4):
                ts(Qm[:, :, h, :], qd16.rearrange("p (a b) -> p a b", a=4), c32[:, C_HM + h:C_HM + h + 1], None, ALU.mult, None,
                   [b_qd, b_c32], [b_Qm])
            yield
            act(Eb, Eb, AF.Ln, [b_Eb], [b_Eb], bias=1.0)
            act(Eb, Eb, AF.Exp, [b_Eb], [b_Eb], scale=-1.0)
            tt(R, R, Eb, ALU.mult, [b_R, b_Eb], [b_R])
            ggl = c32[:, C_GGLA + l * 256:C_GGLA + (l + 1) * 256]
            tt(R, R, ggl.unsqueeze(1).to_broadcast([128, 4, 256]), ALU.mult, [b_R, b_c32], [b_R])
            ps7b = PS[7][:, 0:256].bitcast(BF16)
            S.group("pe", [lambda e, s=s: e.transpose(ps7b[:, s * 128:(s + 1) * 128], ki16[:, s * 128:(s + 1) * 128], identb)
                           for s in range(4)], [b_ki, b_c16], [PSB[7]])
            cp(kitok, ps7b.rearrange("p (a b) -> p a b", a=4), [PSB[7]], [b_kitok], eng="act")
            yield
            ps6b = PS[6][:, 256:512].bitcast(BF16)
            for s in range(4):
                mm(PS[5][:, :], [(ki16[:, s * 128:(s + 1) * 128], Qm[:, s, :, :])], [b_ki, b_Qm], [PSB[5]])
                mm(PS[7][:, 256:512], [(kitok[:, s, :], gv16[:, s, :])], [b_kitok, b_gv], [PSB[7]])
                tt(attm, PS[5][:, :], c16[:, K_GM:K_GM + 512], ALU.mult, [PSB[5], b_c16], [b_attm])
                tt(stmp, S32, PS[7][:, 256:512], ALU.add, [b_S32, PSB[7]], [b_stmp])
                yield
                fns = [lambda e, s=s: e.matmul(PS[6][:, 0:256], qd16[:, s * 128:(s + 1) * 128], Sbf, start=True, stop=False)]
                for h in range(4):
                    fns.append(lambda e, s=s, h=h: e.matmul(PS[6][:, h * 64:(h + 1) * 64], attm[:, h * 128:(h + 1) * 128],
                                                            gv16[:, s, h * 64:(h + 1) * 64], start=False, stop=(h == 3)))
                S.group("pe", fns, [b_qd, b_Sbf, b_attm, b_gv], [PSB[6]])
                cp(o32, PS[6][:, 0:256], [PSB[6]], [b_o32], eng="act")
                stt(S32, stmp, eq[:, s * 128 + 127:s * 128 + 128], c32[:, C_BD:C_BD + 256], ALU.mult, ALU.mult,
                    [b_stmp, b_eq, b_c32], [b_S32])
                cp(Sbf, S32, [b_S32], [b_Sbf], eng="act")
                tt(osq, o32, o32, ALU.mult, [b_o32], [b_osq])
                S.op("dve", lambda e: e.tensor_reduce(ssq4, osq.rearrange("p (a b) -> p a b", a=4), AX.X, ALU.add), [b_osq], [b_ssq4])
                act(ssq4, ssq4, AF.Ln, [b_ssq4], [b_ssq4], bias=EPS, scale=1.0 / 64.0)
                act(ssq4, ssq4, AF.Exp, [b_ssq4], [b_ssq4], scale=-0.5)
                tt(t1.rearrange("p (a b) -> p a b", a=4), o32.rearrange("p (a b) -> p a b", a=4),
                   ssq4.unsqueeze(2).to_broadcast([128, 4, 64]), ALU.mult, [b_o32, b_ssq4], [b_t1])
                tt(y16, t1, R[:, s, :], ALU.mult, [b_t1, b_R], [b_y16])
                yield
                S.group("pe", [lambda e, s=s, c=c: e.transpose(ps6b[:, c * 128:(c + 1) * 128], y16[:, c * 128:(c + 1) * 128], identb)
                               for c in range(2)], [b_y16, b_c16], [PSB[6]])
                cp(mixg[:, :, s * 128:(s + 1) * 128], ps6b[:, 0:256].rearrange("p (a b) -> p a b", a=2), [PSB[6]], [b_mixg], eng="act")
                yield
            S.dma("sp", lambda e, c0=c0: e.dma_start(out=mixT_d[768:1024, c0:c0 + 512].rearrange("(j p) s -> p j s", p=128), in_=mixg),
                  reads=[b_mixg], sembuf=b_mixg)

        def round_robin(gens):
            gens = list(gens)
            while gens:
                for g in list(gens):
                    try:
                        next(g)
                    except StopIteration:
                        gens.remove(g)

        def chain(*gs):
            for g in gs:
                yield from g

        if l == 0:
            load_x(0)
        if DEEP:
            for _ in norm(0):
                pass
        for t in range(NT + 1):
            gens = []
            if t < NT:
                gens.append(projs(t) if DEEP else chain(norm(t), projs(t)))
            if t >= 1:
                gens.append(gla(t - 1))
            if DEEP and t + 1 < NT:
                gens.append(norm(t + 1))
            round_robin(gens)
            if t < NT:
                stage1b(t)

    def phase_B(l):
        kbytes = S_LEN * 2
        KT = [view(WA, 0, [S_LEN], BF16), view(WA, kbytes, [S_LEN], BF16)]
        VV = [view(WA, 2 * kbytes, [NKT, 128], BF16), view(WA, 3 * kbytes, [NKT, 128], BF16)]
        QT = [view(SCR, 2048 * i, [2, 512], BF16) for i in range(3)]
        Pt = [view(SCR, 6144 + 2048 * i, [1024], BF16) for i in range(4)]
        Psum = [view(SCR, 14336 + 2048 * i, [1024], BF16) for i in range(2)]
        Ptmp = view(SCR, 18432, [1024], BF16)
        r1 = view(SCR, 20480, [512])
        r2 = view(SCR, 22528, [512])
        A1 = view(SCR, 24576, [512])
        A2 = view(SCR, 26624, [512])
        ot = view(SCR, 28672, [512])
        osq = view(SCR, 30720, [512], BF16)
        rs = view(SCR, 31744, [512])
        yt = [view(SCR, 33792, [512], BF16), view(SCR, 34816, [512], BF16)]
        b_KT = [S.buf("KT0"), S.buf("KT1")]
        b_VV = [S.buf("VV0"), S.buf("VV1")]
        b_QT = [S.buf(f"QT{i}") for i in range(3)]
        b_P = [S.buf(f"P{i}") for i in range(4)]
        b_Psum = [S.buf("Psum0"), S.buf("Psum1")]
        b_Ptmp = S.buf("Ptmp")
        b_r1, b_r2, b_A1, b_A2, b_ot, b_osq, b_rs = (S.buf(n) for n in ("r1", "r2", "A1", "A2", "ot", "osq", "rs"))
        b_yt = [S.buf("yt0"), S.buf("yt1")]
        nlam = misc[:, M_NLAM + l:M_NLAM + l + 1]

        def load_head(h):
            S.dma("sp", lambda e: e.dma_start(out=KT[h % 2], in_=kT_d[h]), writes=[b_KT[h % 2]], sembuf=b_KT[h % 2])
            S.dma("sp", lambda e: e.dma_start(out=VV[h % 2], in_=v4_d[h]), writes=[b_VV[h % 2]], sembuf=b_VV[h % 2])

        jobs = [(h, qt) for h in range(4) for qt in range(NT)]

        def load_q(ji):
            h, qt = jobs[ji]
            S.dma("sp", lambda e: e.dma_start(out=QT[ji % 3], in_=qT_d[2 * h:2 * h + 2, :, qt * 512:(qt + 1) * 512].rearrange("m p s -> p m s")),
                  writes=[b_QT[ji % 3]], sembuf=b_QT[ji % 3])

        PEA = True
        njobs = len(jobs)
        steps = [(ji, kt) for ji, (h, qt) in enumerate(jobs) for kt in range(4 * qt + 4)]
        NS = len(steps)
        later = {}

        def defer(i, fn):
            later.setdefault(i, []).append(fn)

        def qk(i):
            ji, kt = steps[i]
            h, qt = jobs[ji]
            if kt == 2 and qt == 0 and h + 1 < 4:
                load_head(h + 1)
            if kt == 0 and ji + 2 < njobs:
                load_q(ji + 2)
            K_, Q_ = KT[h % 2], QT[ji % 3]
            for m in range(2):
                bank = 2 * (kt % 2) + m
                mm(PS[bank][:, :], [(K_[:, kt * 128:(kt + 1) * 128], Q_[:, m, :])], [b_KT[h % 2], b_QT[ji % 3]], [PSB[bank]], attach=PEA)

        def expo(i):
            ji, kt = steps[i]
            h, qt = jobs[ji]
            nk = 4 * qt + 4
            b0 = 2 * (kt % 2)
            P_ = Pt[kt % 4]
            bP = b_P[kt % 4]
            act(P_, PSALL[:, b0 * 512:(b0 + 2) * 512], AF.Exp, [PSB[b0], PSB[b0 + 1]], [bP])
            if kt >= 4 * qt:
                r = kt - 4 * qt
                msk = c16[:, K_AM + r * 512:K_AM + (r + 1) * 512].unsqueeze(1).to_broadcast([128, 2, 512])
                P3 = P_.rearrange("p (a b) -> p a b", a=2)
                tt(P3, P3, msk, ALU.mult, [bP, b_c16], [bP])
            g = kt // 4
            if kt % 4 == 1:
                tt(Psum[g % 2], Pt[(kt - 1) % 4], P_, ALU.add, [b_P[(kt - 1) % 4], bP], [b_Psum[g % 2]])
            if kt % 4 == 3 and kt != nk - 1:
                tt(Ptmp, Pt[(kt - 1) % 4], P_, ALU.add, [b_P[(kt - 1) % 4], bP], [b_Ptmp])
                tt(Psum[g % 2], Psum[g % 2], Ptmp, ALU.add, [b_Ptmp, b_Psum[g % 2]], [b_Psum[g % 2]])

        def den_mm(rhs_ap, rbufs, first, last):
            for m in range(2):
                S.group("pe", [lambda e, m=m, first=first, last=last: e.matmul(
                    PS[6 + m][:, :], ones16[:, :], rhs_ap[:, m * 512:(m + 1) * 512], start=first, stop=last)],
                    [b_ones] + rbufs, [PSB[6 + m]], attach=PEA)

        def av(i):
            ji, kt = steps[i]
            h, qt = jobs[ji]
            nk = 4 * qt + 4
            V_ = VV[h % 2]
            P_ = Pt[kt % 4]
            for m in range(2):
                S.group("pe", [lambda e, m=m: e.matmul(
                    PS[4 + m][:, :], V_[:, kt, :], P_[:, m * 512:(m + 1) * 512], start=(kt == 0), stop=(kt == nk - 1))],
                    [b_VV[h % 2], b_P[kt % 4]], [PSB[4 + m]], attach=PEA)
            if kt % 4 == 1 and kt >= 5:
                g = (kt - 5) // 4
                den_mm(Psum[g % 2], [b_Psum[g % 2]], g == 0, False)
            if kt == nk - 1:
                g = nk // 4 - 1
                den_mm(Psum[g % 2], [b_Psum[g % 2]], g == 0, False)
                den_mm(Pt[(kt - 1) % 4], [b_P[(kt - 1) % 4]], False, False)
                den_mm(P_, [b_P[kt % 4]], False, True)
                cp(A1, PS[4][:, :], [PSB[4]], [b_A1], eng="dve")
                act(r1, PS[6][:, :], AF.Ln, [PSB[6]], [b_r1])
                cp(A2, PS[5][:, :], [PSB[5]], [b_A2], eng="dve")
                act(r2, PS[7][:, :], AF.Ln, [PSB[7]], [b_r2])

                def e_a():
                    act(r1, r1, AF.Exp, [b_r1], [b_r1], scale=-1.0)
                    act(r2, r2, AF.Exp, [b_r2], [b_r2], scale=-1.0)

                def e_b():
                    tt(A1, A1, r1, ALU.mult, [b_A1, b_r1], [b_A1])
                    tt(A2, A2, r2, ALU.mult, [b_A2, b_r2], [b_A2])
                    stt(ot, A2, nlam, A1, ALU.mult, ALU.add, [b_A2, b_A1, b_misc], [b_ot])

                def e_c():
                    act(osq, ot, AF.Square, [b_ot], [b_osq])

                def e_d():
                    mm(PS[0][:, :], [(onesV[:, :], osq)], [b_osq, b_ones], [PSB[0]])
                    act(rs, PS[0][:, :], AF.Ln, [PSB[0]], [b_rs], bias=EPS)
                    act(rs, rs, AF.Exp, [b_rs], [b_rs], scale=-0.5)

                def e_e(ji=ji, h=h, qt=qt):
                    y_ = yt[ji % 2]
                    stt(y_, ot, misc[:, M_GD + 4 * l + h:M_GD + 4 * l + h + 1], rs, ALU.mult, ALU.mult, [b_ot, b_rs, b_misc], [b_yt[ji % 2]])
                    S.dma("sp", lambda e: e.dma_start(out=mixT_d[256 + h * 128:256 + (h + 1) * 128, qt * 512:(qt + 1) * 512], in_=y_),
                          reads=[b_yt[ji % 2]], sembuf=b_yt[ji % 2])
                for d_, f_ in enumerate((e_a, e_b, e_c, e_d, e_e)):
                    defer(i + 2 + d_, f_)

        load_head(0)
        load_q(0)
        if njobs > 1:
            load_q(1)
        qk(0)
        for i in range(NS):
            if i + 1 < NS:
                qk(i + 1)
            if i >= 1:
                av(i - 1)
            expo(i)
            for f_ in later.pop(i, []):
                f_()
        av(NS - 1)
        for k_ in sorted(later):
            for f_ in later[k_]:
                f_()

    def phase_C(l):
        g2 = c32[:, C_G2 + l * 8:C_G2 + (l + 1) * 8]
        hA = [view(SCR, 0, [8, 512]), view(SCR, 16384, [8, 512])]
        mx = [view(SCR, 32768, [8, 512], BF16), view(SCR, 40960, [8, 512], BF16)]
        sq = view(WB, 16384, [8, 512], BF16)
        rstd = view(SCR, 51200, [512])
        zT = view(SCR, 53248, [8, 512], BF16)
        b_hA = [S.buf("ChA0"), S.buf("ChA1")]
        b_mx = [S.buf("Cmx0"), S.buf("Cmx1")]
        b_sq = [S.buf(f"Csq{c}") for c in range(8)]
        b_rstd, b_zT = S.buf("Crstd"), S.buf("CzT")

        def load(t):
            i = t % 2
            S.dma("sp", lambda e: e.dma_start(out=hA[i], in_=hT_d[:, t * 512:(t + 1) * 512].rearrange("(k p) s -> p k s", p=128)),
                  writes=[b_hA[i]], sembuf=b_hA[i])
            S.dma("sp", lambda e: e.dma_start(out=mx[i], in_=mixT_d[:, t * 512:(t + 1) * 512].rearrange("(k p) s -> p k s", p=128)),
                  writes=[b_mx[i]], sembuf=b_mx[i])

        load(0)
        for t in range(NT):
            i = t % 2
            if t + 1 < NT:
                load(t + 1)
            for c in range(8):
                bank = c % 4
                mm(PS[bank][:, :], [(WoutV[:, k, c * 128:(c + 1) * 128], mx[i][:, k, :]) for k in range(8)],
                   [b_wout, b_mx[i]], [PSB[bank]])
                tt(hA[i][:, c, :], hA[i][:, c, :], PS[bank][:, :], ALU.add, [PSB[bank], b_hA[i]], [b_hA[i]])
                act(sq[:, c, :], hA[i][:, c, :], AF.Square, [b_hA[i]], [b_sq[c]])
            for c in range(8):
                S.group("pe", [lambda e, c=c: e.matmul(PS[4][:, :], onesD[:, :], sq[:, c, :], start=(c == 0), stop=(c == 7))],
                        [b_sq[c], b_ones], [PSB[4]])
            act(rstd, PS[4][:, :], AF.Ln, [PSB[4]], [b_rstd], bias=EPS)
            act(rstd, rstd, AF.Exp, [b_rstd], [b_rstd], scale=-0.5)
            for c in range(8):
                stt(zT[:, c, :], hA[i][:, c, :], g2[:, c:c + 1], rstd, ALU.mult, ALU.mult, [b_hA[i], b_rstd, b_c32], [b_zT])
            S.dma("sp", lambda e, i=i, t=t: e.dma_start(out=hT_d[:, t * 512:(t + 1) * 512].rearrange("(k p) s -> p k s", p=128), in_=hA[i]),
                  reads=[b_hA[i]], sembuf=b_hA[i])
            S.dma("sp", lambda e, t=t: e.dma_start(out=zT_d[:, t * 512:(t + 1) * 512].rearrange("(k p) s -> p k s", p=128), in_=zT),
                  reads=[b_zT], sembuf=b_zT)

    def phase_D(l, hf, final):
        W1 = W1V[hf]
        W2 = W2V[hf]
        hA = [view(SCR, 0, [8, 512]), view(SCR, 16384, [8, 512])]
        zT = view(SCR, 32768, [8, 512], BF16)
        hid = view(SCR, 40960, [16, 512], BF16)
        rt = [view(SCR, 57344, [512]), view(SCR, 59392, [512])]
        sq = view(SCR, 61440, [2, 512], BF16)
        rstd = view(SCR, 63488, [512])
        ost = [view(WA, 0, [1024]), view(WA, 4096, [1024])]
        b_hA = [S.buf("DhA0"), S.buf("DhA1")]
        b_zT = S.buf("DzT")
        b_hid = [S.buf(f"hid{f}") for f in range(16)]
        b_rt = [S.buf("rt0"), S.buf("rt1")]
        b_sq = [S.buf("Dsq0"), S.buf("Dsq1")]
        b_rstd = S.buf("Drstd")
        b_ost = [S.buf("ost0"), S.buf("ost1")]
        gf = c32[:, C_GF:C_GF + 8]

        def load_h(t):
            i = t % 2
            S.dma("sp", lambda e: e.dma_start(out=hA[i], in_=hT_d[:, t * 512:(t + 1) * 512].rearrange("(k p) s -> p k s", p=128)),
                  writes=[b_hA[i]], sembuf=b_hA[i])

        def load_z(t):
            S.dma("sp", lambda e: e.dma_start(out=zT, in_=zT_d[:, t * 512:(t + 1) * 512].rearrange("(k p) s -> p k s", p=128)),
                  writes=[b_zT], sembuf=b_zT)

        load_h(0)
        load_z(0)
        oi = 0
        for t in range(NT):
            i = t % 2
            if t + 1 < NT:
                load_h(t + 1)
            for f in range(16):
                bank = f % 4
                mm(PS[bank][:, :], [(W1[:, k, f * 128:(f + 1) * 128], zT[:, k, :]) for k in range(8)], [b_w1[hf], b_zT], [PSB[bank]])
                r_ = rt[f % 2]
                act(r_, PS[bank][:, :], AF.Relu, [PSB[bank]], [b_rt[f % 2]])
                tt(hid[:, f, :], r_, PS[bank][:, :], ALU.mult, [b_rt[f % 2], PSB[bank]], [b_hid[f]])
            if t + 1 < NT:
                load_z(t + 1)
            for c in range(8):
                bank = 4 + c % 4
                mm(PS[bank][:, :], [(W2[:, f, c * 128:(c + 1) * 128], hid[:, f, :]) for f in range(16)], [b_w2[hf]] + b_hid, [PSB[bank]])
                tt(hA[i][:, c, :], hA[i][:, c, :], PS[bank][:, :], ALU.add, [PSB[bank], b_hA[i]], [b_hA[i]])
            if not final:
                S.dma("sp", lambda e, i=i, t=t: e.dma_start(out=hT_d[:, t * 512:(t + 1) * 512].rearrange("(k p) s -> p k s", p=128), in_=hA[i]),
                      reads=[b_hA[i]], sembuf=b_hA[i])
            else:
                for c in range(8):
                    act(sq[:, c % 2, :], hA[i][:, c, :], AF.Square, [b_hA[i]], [b_sq[c % 2]])
                    S.group("pe", [lambda e, c=c: e.matmul(PS[0][:, :], onesD[:, :], sq[:, c % 2, :], start=(c == 0), stop=(c == 7))],
                            [b_sq[c % 2], b_ones], [PSB[0]])
                act(rstd, PS[0][:, :], AF.Ln, [PSB[0]], [b_rstd], bias=EPS)
                act(rstd, rstd, AF.Exp, [b_rstd], [b_rstd], scale=-0.5)
                for c in range(8):
                    stt(hA[i][:, c, :], hA[i][:, c, :], gf[:, c:c + 1], rstd, ALU.mult, ALU.mult, [b_hA[i], b_rstd, b_c32], [b_hA[i]])
                for s in range(4):
                    o_ = ost[oi % 2]
                    bo = b_ost[oi % 2]
                    for half in range(2):
                        bank = 1 + half
                        S.group("pe", [lambda e, kk=kk, half=half, bank=bank, s=s, i=i: e.transpose(
                            PS[bank][:, kk * 128:(kk + 1) * 128], hA[i][:, half * 4 + kk, s * 128:(s + 1) * 128], ident32) for kk in range(4)],
                            [b_hA[i], b_c32], [PSB[bank]])
                        cp(o_[:, half * 512:(half + 1) * 512], PS[bank][:, :], [PSB[bank]], [bo], eng="act" if half == 0 else "dve")
                    S.dma("sp", lambda e, o_=o_, t=t, s=s: e.dma_start(out=out_d[t * 512 + s * 128:t * 512 + (s + 1) * 128, :], in_=o_),
                          reads=[bo], sembuf=bo)
                    oi += 1

    def done(tag):
        return stop_after is not None and stop_after == tag

    S.barrier()
    finished = False
    for l in range(n_layers):
        if l > 0:
            load_wout(l)
        phase_A(l)
        S.barrier()
        if done(f"A{l}"):
            break
        phase_B(l)
        S.barrier()
        if done(f"B{l}"):
            break
        load_w12(l, 0)
        phase_C(l)
        S.barrier()
        if done(f"C{l}"):
            break
        load_w12(l, 1)
        phase_D(l, 0, False)
        S.barrier()
        if l + 1 < n_layers:
            load_win(l + 1)
        phase_D(l, 1, l == n_layers - 1)
        S.barrier()
    S.finish()
    return nc, S


def _host_inputs(inp, core_x):
    c32, c16 = _const_tables()
    c32 = _fill_params(c32, inp)
    wg2 = np.ascontiguousarray(np.asarray(inp["gla_w_gate2"], np.float32).transpose(1, 0, 2).reshape(16, L * 128))
    return {
        "x": np.ascontiguousarray(core_x, dtype=np.float32),
        "w_in": np.asarray(inp["w_in"], np.float32),
        "w_out": np.asarray(inp["w_out"], np.float32),
        "w_mlp1": np.asarray(inp["w_mlp1"], np.float32),
        "w_mlp2": np.asarray(inp["w_mlp2"], np.float32),
        "wg2": wg2,
        "pool_w": np.asarray(inp["pool_w"], np.float32),
        "c32": c32,
        "c16": c16,
    }


def kernel(**inputs):
    x = np.asarray(inputs["x"], np.float32)
    B, S_LEN, _ = x.shape
    nc, _ = build_nc(S_LEN)
    base = _host_inputs(inputs, x[0])
    in_maps = []
    for b in range(B):
        m = dict(base)
        m["x"] = np.ascontiguousarray(x[b])
        in_maps.append(m)
    res = run_bass_kernel_spmd(nc, in_maps, core_ids=list(range(B)))
    return np.stack([np.asarray(r["out"], np.float32) for r in res.results], axis=0)
```

```python
import math
import numpy as np
import concourse.bass as bass
import concourse.mybir as mybir
from concourse.bass_utils import run_bass_kernel_spmd

F32 = mybir.dt.float32
BF16 = mybir.dt.bfloat16
ALU = mybir.AluOpType
AF = mybir.ActivationFunctionType
AX = mybir.AxisListType

L = 2
D = 1024
DIN = 2576
DFF = 4096
EPS = 1e-6
SEM_ROT = 24000
import os
SKIP = set(os.environ.get('K_SKIP', '').split(','))
ATTACH = os.environ.get('K_ATTACH', '1') == '1'


class Buf:
    __slots__ = ("name", "w", "r", "dsem", "dcnt", "excl")

    def __init__(self, name):
        self.name = name
        self.excl = False
        self.w = []
        self.r = []
        self.dsem = None
        self.dcnt = 0


class Eng:
    def __init__(self, name):
        self.name = name
        self.sem = None
        self.cnt = 0
        self.seen = {}
        self.recs = []


def _merge(evts):
    d = {}
    for (s, v) in evts:
        k = id(s)
        if k not in d or d[k][1] < v:
            d[k] = (s, v)
    return list(d.values())


class Sched:
    def __init__(self, nc):
        self.nc = nc
        self.engs = {n: Eng(n) for n in ("pe", "act", "dve", "pool", "sp")}
        self.nsem = 0
        self.ninst = 0
        self.dbufs = []
        self.cache = {}
        self.engsems = set()

    def new_sem(self, name):
        self.nsem += 1
        sm = self.nc.alloc_semaphore(name=f"{name}_{self.nsem}")
        if name.startswith("p_"):
            self.engsems.add(id(sm))
        return sm

    def buf(self, name):
        if name not in self.cache:
            self.cache[name] = Buf(name)
        return self.cache[name]

    def _deps(self, reads, writes):
        ev = []
        for b in reads:
            ev.extend(b.w)
        for b in writes:
            ev.extend(b.w)
            ev.extend(b.r)
        return _merge(ev)

    def _need(self, eng, evts):
        waits = []
        for (s, v) in evts:
            k = id(s)
            if k in eng.seen and eng.seen[k][1] >= v:
                continue
            eng.seen[k] = (s, v)
            waits.append((s, v))
        return waits

    def _commit(self, ev, reads, writes):
        for b in writes:
            b.w = [ev]
            b.r = []
        for b in reads:
            if b in writes:
                continue
            b.r = [e for e in b.r if e[0] is not ev[0]] + [ev]

    def _eng_ev(self, eng):
        if eng.sem is None or eng.cnt >= SEM_ROT:
            eng.sem = self.new_sem("p_" + eng.name)
            eng.cnt = 0
        eng.cnt += 1
        return (eng.sem, eng.cnt)

    def op(self, engname, fn, reads=(), writes=()):
        return self.group(engname, [fn], reads, writes)

    def group(self, engname, fns, reads=(), writes=(), attach=None):
        eng = self.engs[engname]
        if attach is None:
            attach = ATTACH
        xr = [b for b in reads if b.excl]
        if xr:
            reads = [b for b in reads if not b.excl]
            writes = list(writes) + [b for b in xr if b not in writes]
        rdeps = self._deps(reads, ())
        waits = self._need(eng, self._deps(reads, writes))
        ev = self._eng_ev(eng)
        aw = None
        if attach and ATTACH:
            rd = {id(s_): v_ for (s_, v_) in rdeps}
            for w_ in reversed(waits):
                if id(w_[0]) not in self.engsems:
                    continue
                if engname == "pe" and id(w_[0]) in rd and rd[id(w_[0])] >= w_[1]:
                    continue
                aw = w_
                break
            if aw is not None:
                waits = [w_ for w_ in waits if w_ is not aw]
        eng.recs.append((waits, list(fns), (ev[0], 1, False), aw))
        self._commit(ev, reads, writes)
        self.ninst += len(fns)
        return ev

    def dma(self, qname, fns, reads=(), writes=(), sembuf=None):
        if not isinstance(fns, (list, tuple)):
            fns = [fns]
        eng = self.engs[qname]
        sb = sembuf
        if sb.dsem is None or sb.dcnt + 16 * len(fns) >= SEM_ROT:
            sb.dsem = self.new_sem("d_" + sb.name)
            sb.dcnt = 0
            self.dbufs.append(sb)
        waits = self._need(eng, self._deps(reads, writes))
        sb.dcnt += 16 * len(fns)
        ev = (sb.dsem, sb.dcnt)
        eng.recs.append((waits, list(fns), (sb.dsem, 16, True)))
        self._commit(ev, reads, writes)
        self.ninst += len(fns)
        return ev

    def barrier(self):
        ev = []
        for e in self.engs.values():
            if e.sem is not None:
                ev.append((e.sem, e.cnt))
        seen_ids = set()
        for b in self.dbufs:
            if b.dsem is not None and id(b.dsem) not in seen_ids:
                seen_ids.add(id(b.dsem))
                ev.append((b.dsem, b.dcnt))
        ev = _merge(ev)
        for e in self.engs.values():
            waits = self._need(e, ev)
            if waits:
                e.recs.append((waits, [], None))

    def finish(self):
        self.barrier()
        nc = self.nc

        def replay(eng):
            def run(e):
                for rec in eng.recs:
                    waits, fns, inc = rec[0], rec[1], rec[2]
                    aw = rec[3] if len(rec) > 3 else None
                    for (s, v) in waits:
                        e.wait_ge(s, v)
                    last = None
                    for f in fns:
                        last = f(e)
                        if aw is not None:
                            last._wait_ge(aw[0], aw[1])
                            aw = None
                        if inc is not None and inc[2]:
                            last.then_inc(inc[0], inc[1])
                    if inc is not None and not inc[2] and last is not None:
                        last.then_inc(inc[0], inc[1])
            return run

        with nc.Block() as block:
            block.tensor(replay(self.engs["pe"]))
            block.scalar(replay(self.engs["act"]))
            block.vector(replay(self.engs["dve"]))
            block.gpsimd(replay(self.engs["pool"]))
            block.sync(replay(self.engs["sp"]))


C_ID = 0
C_BD = 128
C_HM = 384
C_CORR = 388
C_G1 = 420
C_G2 = C_G1 + L * 8
C_GF = C_G2 + L * 8
C_PSC = C_GF + 8
C_GD = C_PSC + L * 2
C_BG = C_GD + L * 4
C_GGLA = C_BG + L
C_LQ = C_GGLA + L * 256
NC32 = C_LQ + L * 256
K_AM = 0
K_GM = 2048
K_IDB = 2560
NC16 = 2688

POOL_WINS = (2, 4, 8, 16)


def _const_tables():
    c32 = np.zeros((128, NC32), np.float32)
    c32[:, C_ID:C_ID + 128] = np.eye(128, dtype=np.float32)
    p = np.arange(128)
    c = np.arange(256)
    c32[:, C_BD:C_BD + 256] = (p[:, None] // 32 == c[None, :] // 64).astype(np.float32)
    c32[:, C_HM:C_HM + 4] = (p[:, None] // 32 == np.arange(4)[None, :]).astype(np.float32)
    corr = np.zeros((128, 2, 16), np.float32)
    for j in range(2):
        for half in range(2):
            w = POOL_WINS[2 * j + half]
            i = np.arange(16)
            corr[half * 64:(half + 1) * 64, j, :] = w / np.minimum(i + 1, w)
    c32[:, C_CORR:C_CORR + 32] = corr.reshape(128, 32)
    c16 = np.zeros((128, NC16), np.float32)
    jj = np.arange(128)
    ii = np.arange(512)
    for r in range(4):
        c16[:, K_AM + r * 512:K_AM + (r + 1) * 512] = (
            (2 * r + jj[:, None] // 64) <= (ii[None, :] // 64)).astype(np.float32)
    gm = (jj[:, None] <= np.arange(128)[None, :]).astype(np.float32)
    c16[:, K_GM:K_GM + 512] = np.tile(gm, (1, 4))
    c16[:, K_IDB:K_IDB + 128] = np.eye(128, dtype=np.float32)
    return c32, c16


def _fill_params(c32, inp):
    def cols(v, n):
        return np.ascontiguousarray(np.asarray(v, np.float32).reshape(n, 128).T)
    for l in range(L):
        c32[:, C_G1 + l * 8:C_G1 + (l + 1) * 8] = cols(inp["norm1_g"][l], 8)
        c32[:, C_G2 + l * 8:C_G2 + (l + 1) * 8] = cols(inp["norm2_g"][l], 8)
        c32[:, C_PSC + l * 2:C_PSC + (l + 1) * 2] = cols(inp["pool_scale"][l], 2)
        c32[:, C_GD + l * 4:C_GD + (l + 1) * 4] = cols(inp["diff_norm_g"][l], 4)
        c32[:, C_BG + l:C_BG + l + 1] = cols(inp["gla_b_gate"][l], 1)
        c32[:, C_GGLA + l * 256:C_GGLA + (l + 1) * 256] = np.asarray(inp["gla_norm_g"][l], np.float32)[None, :]
        for i, nm in enumerate(("diff_lq1", "diff_lk1", "diff_lq2", "diff_lk2")):
            c32[:, C_LQ + l * 256 + i * 64:C_LQ + l * 256 + (i + 1) * 64] = np.asarray(inp[nm][l], np.float32)[None, :]
    c32[:, C_GF:C_GF + 8] = cols(inp["final_norm_g"], 8)
    return c32


def build_nc(S_LEN, debug=False, n_layers=L, stop_after=None):
    NT = S_LEN // 512
    NKT = S_LEN // 128
    nc = bass.Bass("TRN2", target_bir_lowering=False)
    S = Sched(nc)

    def din(name, shape, dt=F32):
        return nc.dram_tensor(name, list(shape), dt, kind="ExternalInput").ap()

    x_d = din("x", [S_LEN, D])
    win_d = din("w_in", [L, D, DIN])
    wout_d = din("w_out", [L, D, D])
    w1_d = din("w_mlp1", [L, D, DFF])
    w2_d = din("w_mlp2", [L, DFF, D])
    wg2_d = din("wg2", [16, L * 128])
    poolw_d = din("pool_w", [L, 4, 64, 64])
    c32_d = din("c32", [128, NC32])
    c16_d = din("c16", [128, NC16])
    out_d = nc.dram_tensor("out", [S_LEN, D], F32, kind="ExternalOutput").ap()
    skind = "ExternalOutput" if debug else "Internal"

    def dscr(name, shape, dt):
        return nc.dram_tensor(name, list(shape), dt, kind=skind).ap()

    hT_d = dscr("hT", [D, S_LEN], F32)
    qT_d = dscr("qT", [8, 128, S_LEN], BF16)
    kT_d = dscr("kT", [4, 128, S_LEN], BF16)
    v4_d = dscr("v4", [4, 128, NKT, 128], BF16)
    mixT_d = dscr("mixT", [D, S_LEN], BF16)
    zT_d = dscr("zT", [D, S_LEN], BF16)

    def sbt(name, cols, dt=F32):
        return nc.sbuf_tensor(name, [128, cols], dt).__enter__()

    WA = sbt("WA", 16384)
    WB = sbt("WB", 16384)
    SCR = sbt("SCR", 16384)
    c32 = sbt("c32s", NC32)
    c16 = sbt("c16s", NC16, BF16)
    wg2 = sbt("wg2s", L * 128)
    poolW = sbt("poolW", L * 256, BF16)
    misc = sbt("misc", 64)
    ones16 = sbt("ones16", 128, BF16)
    onesD = sbt("onesD", 128, BF16)
    onesV = sbt("onesV", 128, BF16)
    ones32 = sbt("ones32", 128)
    PSALL = nc.psum_tensor("psall", [128, 4096], F32).__enter__()
    PS = [PSALL[:, i * 512:(i + 1) * 512] for i in range(8)]
    PSB = [S.buf(f"ps{i}") for i in range(8)]
    for b_ in PSB:
        b_.excl = True

    def view(t, off, shape, dt=F32):
        n = 1
        for s_ in shape:
            n *= s_
        nbytes = n * (4 if dt == F32 else 2)
        assert off % 4 == 0 and nbytes % 4 == 0 and off + nbytes <= 65536, (off, nbytes)
        ap = t[:, off // 4:(off + nbytes) // 4]
        if dt != F32:
            ap = ap.bitcast(dt)
        if len(shape) == 2:
            ap = ap.rearrange("p (a b) -> p a b", a=shape[0])
        elif len(shape) == 3:
            ap = ap.rearrange("p (a b c) -> p a b c", a=shape[0], b=shape[1])
        return ap

    def act(out, in_, func, reads, writes, **kw):
        S.op("act", lambda e: e.activation(out, in_, func, **kw), reads, writes)

    def tt(out, a, b, op, reads, writes, eng="dve"):
        S.op(eng, lambda e: e.tensor_tensor(out, a, b, op), reads, writes)

    def stt(out, in0, scalar, in1, op0, op1, reads, writes, eng="dve"):
        S.op(eng, lambda e: e.scalar_tensor_tensor(out, in0, scalar, in1, op0, op1), reads, writes)

    def ts(out, in0, s1, s2, op0, op1, reads, writes, eng="dve"):
        if op1 is None:
            S.op(eng, lambda e: e.tensor_scalar(out, in0, s1, None, op0), reads, writes)
        else:
            S.op(eng, lambda e: e.tensor_scalar(out, in0, s1, s2, op0, op1), reads, writes)

    def cp(out, in_, reads, writes, eng="dve"):
        if eng == "act":
            S.op("act", lambda e: e.activation(out, in_, AF.Copy), reads, writes)
        else:
            S.op(eng, lambda e: e.tensor_copy(out, in_), reads, writes)

    def mm(out, pairs, reads, writes, start=True, attach=False):
        n = len(pairs)
        fns = []
        for i, (lt, rh) in enumerate(pairs):
            fns.append(lambda e, lt=lt, rh=rh, i=i: e.matmul(out, lt, rh, start=(start and i == 0), stop=(i == n - 1)))
        S.group("pe", fns, reads, writes, attach=attach)

    def rsqrt_inplace(t_ap, buf, scale=1.0):
        act(t_ap, t_ap, AF.Ln, [buf], [buf], bias=EPS, scale=scale)
        act(t_ap, t_ap, AF.Exp, [buf], [buf], scale=-0.5)

    b_c32, b_c16, b_wg2, b_poolW, b_misc, b_ones = (S.buf(n) for n in ("c32", "c16", "wg2", "poolW", "misc", "ones"))
    S.dma("sp", lambda e: e.dma_start(out=c32[:], in_=c32_d), writes=[b_c32], sembuf=b_c32)
    S.op("dve", lambda e: e.memset(wg2[:], 0.0), writes=[b_wg2])
    S.dma("sp", lambda e: e.dma_start(out=wg2[112:128, :], in_=wg2_d), writes=[b_wg2], sembuf=b_wg2)
    S.dma("pool", lambda e: e.dma_start(out=c16[:], in_=c16_d), writes=[b_c16], sembuf=b_c16)
    S.op("dve", lambda e: e.memset(poolW[:], 0.0), writes=[b_poolW])
    pw_fns = []
    for l in range(L):
        for g in range(4):
            j, half = g // 2, g % 2
            pw_fns.append(lambda e, l=l, g=g, j=j, half=half: e.dma_start(
                out=poolW[half * 64:(half + 1) * 64, l * 256 + j * 128 + half * 64: l * 256 + j * 128 + half * 64 + 64],
                in_=poolw_d[l, g]))
    S.dma("pool", pw_fns, writes=[b_poolW], sembuf=b_poolW)
    S.op("dve", lambda e: e.memset(ones16[:], 1.0), writes=[b_ones])
    S.op("dve", lambda e: e.memset(onesD[:], 1.0 / 1024.0), writes=[b_ones])
    S.op("dve", lambda e: e.memset(onesV[:], 1.0 / 128.0), writes=[b_ones])
    S.op("dve", lambda e: e.memset(ones32[:], 1.0), writes=[b_ones])
    ident32 = c32[:, C_ID:C_ID + 128]
    identb = c16[:, K_IDB:K_IDB + 128]
    M_NLAM, M_NBG, M_GD = 0, 4, 8
    for l in range(L):
        lam_init = 0.8 - 0.6 * math.exp(-0.3 * l)
        lq = c32[:, C_LQ + l * 256:C_LQ + (l + 1) * 256]
        tmp = misc[:, 32:34]
        prod = view(SCR, 0, [256])
        b_tmp = S.buf("lamtmp")
        tt(prod[:, 0:64], lq[:, 0:64], lq[:, 64:128], ALU.mult, [b_c32], [b_tmp])
        tt(prod[:, 64:128], lq[:, 128:192], lq[:, 192:256], ALU.mult, [b_c32], [b_tmp])
        S.op("dve", lambda e, prod=prod, tmp=tmp: e.tensor_reduce(
            tmp, prod[:, 0:128].rearrange("p (a b) -> p a b", a=2), AX.X, ALU.add), [b_tmp], [b_misc])
        act(tmp, tmp, AF.Exp, [b_misc], [b_misc])
        stt(misc[:, M_NLAM + l:M_NLAM + l + 1], tmp[:, 1:2], -lam_init, tmp[:, 0:1], ALU.add, ALU.subtract, [b_misc], [b_misc])
        ts(misc[:, M_NBG + l:M_NBG + l + 1], c32[:, C_BG + l:C_BG + l + 1], -1.0, None, ALU.mult, None, [b_c32], [b_misc])
        ts(misc[:, M_GD + 4 * l:M_GD + 4 * l + 4], c32[:, C_GD + 4 * l:C_GD + 4 * l + 4], 1.0 - lam_init, None, ALU.mult, None,
           [b_c32], [b_misc])

    b_win, b_wout = S.buf("win"), S.buf("wout")
    b_w1 = [S.buf("w1a"), S.buf("w1b")]
    b_w2 = [S.buf("w2a"), S.buf("w2b")]
    WinV = view(WA, 0, [8, DIN], BF16)
    WoutV = view(WB, 0, [8, D], BF16)
    W1V = [view(WA, 0, [8, 2048], BF16), view(WB, 0, [8, 2048], BF16)]
    W2V = [view(WA, 32768, [16, D], BF16), view(WB, 32768, [16, D], BF16)]

    def load_win(l):
        S.dma("pool", [lambda e, kc=kc: e.dma_start(out=WinV[:, kc, :], in_=win_d[l, kc * 128:(kc + 1) * 128, :])
                       for kc in range(8)], writes=[b_win], sembuf=b_win)

    def load_wout(l):
        S.dma("pool", [lambda e, kc=kc: e.dma_start(out=WoutV[:, kc, :], in_=wout_d[l, kc * 128:(kc + 1) * 128, :])
                       for kc in range(8)], writes=[b_wout], sembuf=b_wout)

    def load_w12(l, hf):
        S.dma("pool", [lambda e, kc=kc: e.dma_start(out=W1V[hf][:, kc, :],
                                                    in_=w1_d[l, kc * 128:(kc + 1) * 128, hf * 2048:(hf + 1) * 2048])
                       for kc in range(8)], writes=[b_w1[hf]], sembuf=b_w1[hf])
        S.dma("pool", [lambda e, f=f: e.dma_start(out=W2V[hf][:, f, :],
                                                  in_=w2_d[l, (hf * 16 + f) * 128:(hf * 16 + f + 1) * 128, :])
                       for f in range(16)], writes=[b_w2[hf]], sembuf=b_w2[hf])

    load_win(0)
    load_wout(0)

    def phase_A(l):
        g1 = c32[:, C_G1 + l * 8:C_G1 + (l + 1) * 8]
        hA = view(SCR, 0, [8, 512])
        uTs = [view(SCR, 16384, [8, 512], BF16), view(WA, 43008, [8, 512], BF16)]
        xs = view(SCR, 24576, [2, 1024])
        sq = view(SCR, 32768, [2, 512], BF16)
        rstd = view(SCR, 34816, [512])
        pE = view(SCR, 36864, [2, 528])
        pX = view(SCR, 41088, [2, 528])
        pY = view(SCR, 45312, [2, 528])
        pM = view(SCR, 49536, [2, 512])
        pL = view(SCR, 53632, [2, 512], BF16)
        mixp = view(SCR, 55680, [2, 512], BF16)
        mixg = view(SCR, 57728, [2, 512], BF16)
        gg32 = view(SCR, 59776, [512])
        S32 = view(SCR, 61824, [256])
        Sbf = view(SCR, 62848, [256], BF16)
        stmp = view(SCR, 63360, [256])
        ssq4 = view(SCR, 64384, [4])
        qst = view(WB, 16384, [8, 512], BF16)
        kst = view(WB, 24576, [4, 512], BF16)
        vst = view(WB, 28672, [4, 512], BF16)
        gq32 = view(WB, 32768, [512])
        gk32 = view(WB, 34816, [512])
        la = view(WB, 36864, [512])
        Bn = view(WB, 38912, [512])
        eq = view(WB, 40960, [512])
        ek = view(WB, 43008, [512])
        qd16 = view(WB, 45056, [512], BF16)
        ki16 = view(WB, 46080, [512], BF16)
        Qm = view(WB, 47104, [4, 4, 128], BF16)
        kitok = view(WB, 51200, [4, 128], BF16)
        gv16 = view(WB, 52224, [4, 256], BF16)
        R = view(WB, 54272, [4, 256])
        Eb = view(WB, 58368, [4, 256])
        attm = view(WB, 62464, [512], BF16)
        o32 = view(WB, 63488, [256])
        osq = view(WB, 64512, [256])
        t1 = view(WA, 41216, [256])
        y16 = view(WA, 42240, [256], BF16)
        attm_l = [attm, view(WA, 59392, [512], BF16)]
        o32_l = [o32, view(WA, 60416, [256])]
        osq_l = [osq, view(WA, 61440, [256])]
        t1_l = [t1, view(WA, 62464, [256])]
        y16_l = [y16, view(WA, 63488, [256], BF16)]
        ssq4_l = [ssq4, view(WA, 64000, [4])]

        b_hA = [S.buf(f"hA{k}") for k in range(8)]
        b_uTs = [[S.buf(f"uT{k}") for k in range(8)], [S.buf(f"uTb{k}") for k in range(8)]]
        DEEP = False

        def upar(t):
            return (t % 2) if DEEP else 0
        b_xs = [S.buf("xs0"), S.buf("xs1")]
        b_sq = [S.buf("sq0"), S.buf("sq1")]
        b_rstd = S.buf("rstd")
        b_pE, b_pX, b_pY, b_pM, b_pL = (S.buf(n) for n in ("pE", "pX", "pY", "pM", "pL"))
        b_mixp, b_mixg = S.buf("mixp"), S.buf("mixg")
        b_gg, b_S32, b_Sbf, b_stmp, b_ssq4 = (S.buf(n) for n in ("gg", "S32", "Sbf", "stmp", "ssq4"))
        b_qst, b_kst, b_vst = S.buf("qst"), S.buf("kst"), S.buf("vst")
        b_gq, b_gk, b_la, b_Bn, b_eq, b_ek = (S.buf(n) for n in ("gq", "gk", "la", "Bn", "eq", "ek"))
        b_qd, b_ki, b_Qm, b_kitok, b_gv, b_R, b_Eb = (S.buf(n) for n in ("qd", "ki", "Qm", "kitok", "gv", "R", "Eb"))
        b_attm, b_o32, b_osq, b_t1, b_y16 = (S.buf(n) for n in ("attm", "o32", "osq", "t1", "y16"))
        b_attm_l = [b_attm, S.buf("attm2")]
        b_o32_l = [b_o32, S.buf("o32b")]
        b_osq_l = [b_osq, S.buf("osqb")]
        b_t1_l = [b_t1, S.buf("t1b")]
        b_y16_l = [b_y16, S.buf("y16b")]
        b_ssq4_l = [b_ssq4, S.buf("ssq4b")]
        b_hst = S.buf("hst")

        S.op("dve", lambda e: e.memset(qst, 0.0), writes=[b_qst])
        S.op("dve", lambda e: e.memset(pE[:, :, 0:16], 0.0), writes=[b_pE])
        S.op("dve", lambda e: e.memset(S32, 0.0), writes=[b_S32])
        S.op("dve", lambda e: e.memset(Sbf, 0.0), writes=[b_Sbf])
        projbanks = [3, 4]
        pbi = [0]

        def nextbank():
            b = projbanks[pbi[0] % len(projbanks)]
            pbi[0] += 1
            return b

        def load_tile(t):
            if l == 0:
                return
            for kc in range(8):
                S.dma("sp", lambda e, kc=kc: e.dma_start(out=hA[:, kc, :], in_=hT_d[kc * 128:(kc + 1) * 128, t * 512:(t + 1) * 512]),
                      writes=[b_hA[kc]], sembuf=b_hA[kc])

        if l > 0:
            load_tile(0)
        xs4 = view(WA, 43008, [4, 1024])
        b_xs4 = [S.buf(f"xs4_{i}") for i in range(4)]

        def load_x(t):
            for s in range(4):
                S.dma("sp", lambda e, s=s, t=t: e.dma_start(out=xs4[:, s, :], in_=x_d[t * 512 + s * 128:t * 512 + (s + 1) * 128, :]),
                      writes=[b_xs4[s]], sembuf=b_xs4[s])

        cur = [0]

        def proj_fm(col0, M):
            bank = nextbank()
            uT = uTs[cur[0]]
            mm(PS[bank][0:M, :], [(WinV[:, kc, col0:col0 + M], uT[:, kc, :]) for kc in range(8)],
               b_uTs[cur[0]] + [b_win], [PSB[bank]])
            return bank

        def proj_tm(col0, s):
            bank = nextbank()
            uT = uTs[cur[0]]
            mm(PS[bank][:, :], [(uT[:, kc, s * 128:(s + 1) * 128], WinV[:, kc, col0:col0 + 512]) for kc in range(8)],
               b_uTs[cur[0]] + [b_win], [PSB[bank]])
            return bank

        def norm(t):
            c0 = t * 512
            uT = uTs[upar(t)]
            b_uT = b_uTs[upar(t)]
            if l == 0:
                for s in range(4):
                    for half in range(2):
                        bank = half
                        fns = []
                        for kk in range(4):
                            kc = half * 4 + kk
                            fns.append(lambda e, kc=kc, kk=kk, s=s, bank=bank: e.transpose(
                                PS[bank][:, kk * 128:(kk + 1) * 128], xs4[:, s, kc * 128:(kc + 1) * 128], ident32))
                        S.group("pe", fns, [b_xs4[s], b_c32], [PSB[bank]])
                        outv = hA[:, half * 4:(half + 1) * 4, s * 128:(s + 1) * 128]
                        inv = PS[bank][:, :].rearrange("p (a b) -> p a b", a=4)
                        wr = [b_hA[half * 4 + kk] for kk in range(4)]
                        cp(outv, inv, [PSB[bank]], wr, eng="act" if half == 0 else "dve")
                    yield
                if t + 1 < NT:
                    load_x(t + 1)
                S.dma("sp", [lambda e, kc=kc, c0=c0: e.dma_start(out=hT_d[kc * 128:(kc + 1) * 128, c0:c0 + 512], in_=hA[:, kc, :])
                             for kc in range(8)], reads=b_hA, sembuf=b_hst)
            for kc in range(8):
                act(sq[:, kc % 2, :], hA[:, kc, :], AF.Square, [b_hA[kc]], [b_sq[kc % 2]])
                S.group("pe", [lambda e, kc=kc: e.matmul(PS[2][:, :], onesD[:, :], sq[:, kc % 2, :], start=(kc == 0), stop=(kc == 7))],
                        [b_sq[kc % 2], b_ones], [PSB[2]])
                if kc % 2 == 1:
                    yield
            act(rstd, PS[2][:, :], AF.Ln, [PSB[2]], [b_rstd], bias=EPS)
            act(rstd, rstd, AF.Exp, [b_rstd], [b_rstd], scale=-0.5)
            for kc in range(8):
                stt(uT[:, kc, :], hA[:, kc, :], g1[:, kc:kc + 1], rstd, ALU.mult, ALU.mult,
                    [b_hA[kc], b_rstd, b_c32], [b_uT[kc]])
            if l > 0 and t + 1 < NT:
                load_tile(t + 1)
            yield

        def projs(t):
            c0 = t * 512
            for j in range(2):
                cur[0] = upar(t)
                bk = proj_fm(j * 128, 128)
                cp(pE[:, j, 16:528], PS[bk][:, :], [PSB[bk]], [b_pE], eng="act")
                yield
            for h in range(4):
                cur[0] = upar(t)
                bk = proj_fm(256 + h * 128, 128)
                act(qst[0:64, 2 * h, :], PS[bk][0:64, :], AF.Copy, [PSB[bk]], [b_qst], scale=0.125)
                act(qst[64:128, 2 * h + 1, :], PS[bk][64:128, :], AF.Copy, [PSB[bk]], [b_qst], scale=0.125)
                yield
            S.dma("sp", lambda e, c0=c0: e.dma_start(out=qT_d[:, :, c0:c0 + 512].rearrange("a p s -> p a s"), in_=qst),
                  reads=[b_qst], sembuf=b_qst)
            PM = "dve"
            tt(pX[:, :, 1:528], pE[:, :, 1:528], pE[:, :, 0:527], ALU.add, [b_pE], [b_pX], eng=PM)
            tt(pY[64:128, 0, 3:528], pX[64:128, 0, 3:528], pX[64:128, 0, 1:526], ALU.add, [b_pX], [b_pY], eng=PM)
            tt(pY[:, 1, 3:528], pX[:, 1, 3:528], pX[:, 1, 1:526], ALU.add, [b_pX], [b_pY], eng=PM)
            tt(pX[:, 1, 7:528], pY[:, 1, 7:528], pY[:, 1, 3:524], ALU.add, [b_pY], [b_pX], eng=PM)
            tt(pY[64:128, 1, 15:528], pX[64:128, 1, 15:528], pX[64:128, 1, 7:520], ALU.add, [b_pX], [b_pY], eng=PM)
            for h in range(4):
                cur[0] = upar(t)
                bk = proj_fm(768 + h * 128, 128)
                cp(kst[:, h, :], PS[bk][:, :], [PSB[bk]], [b_kst], eng="act")
                yield
            S.dma("sp", lambda e, c0=c0: e.dma_start(out=kT_d[:, :, c0:c0 + 512].rearrange("a p s -> p a s"), in_=kst),
                  reads=[b_kst], sembuf=b_kst)
            ts(pM[0:64, 0, :], pX[0:64, 0, 16:528], 0.5, None, ALU.mult, None, [b_pX], [b_pM], eng=PM)
            ts(pM[64:128, 0, :], pY[64:128, 0, 16:528], 0.25, None, ALU.mult, None, [b_pY], [b_pM], eng=PM)
            ts(pM[0:64, 1, :], pX[0:64, 1, 16:528], 0.125, None, ALU.mult, None, [b_pX], [b_pM], eng=PM)
            ts(pM[64:128, 1, :], pY[64:128, 1, 16:528], 0.0625, None, ALU.mult, None, [b_pY], [b_pM], eng=PM)
            if t == 0:
                corr = c32[:, C_CORR:C_CORR + 32].rearrange("p (a b) -> p a b", a=2)
                tt(pM[:, :, 0:16], pM[:, :, 0:16], corr, ALU.mult, [b_pM, b_c32], [b_pM], eng=PM)
            tt(pL, pM, pE[:, :, 16:528], ALU.subtract, [b_pM, b_pE], [b_pL], eng=PM)
            cp(pE[:, :, 0:16], pE[:, :, 512:528], [b_pE], [b_pE], eng=PM)
            for s in range(4):
                cur[0] = upar(t)
                bk = proj_tm(1280, s)
                cp(vst[:, s, :], PS[bk][:, :], [PSB[bk]], [b_vst], eng="act" if s % 2 == 0 else "dve")
                yield
            S.dma("sp", [lambda e, s=s, t=t: e.dma_start(out=v4_d[:, :, 4 * t + s, :].rearrange("h p d -> p h d"),
                                                    in_=vst[:, s, :].rearrange("p (h d) -> p h d", h=4))
                         for s in range(4)], reads=[b_vst], sembuf=b_vst)
            for j in range(2):
                bk = nextbank()
                mm(PS[bk][:, :], [(poolW[:, l * 256 + j * 128:l * 256 + (j + 1) * 128], pL[:, j, :])], [b_pL, b_poolW], [PSB[bk]])
                ts(mixp[:, j, :], PS[bk][:, :], c32[:, C_PSC + 2 * l + j:C_PSC + 2 * l + j + 1], None, ALU.mult, None,
                   [PSB[bk], b_c32], [b_mixp])
            S.dma("sp", lambda e, c0=c0: e.dma_start(out=mixT_d[0:256, c0:c0 + 512].rearrange("(j p) s -> p j s", p=128), in_=mixp),
                  reads=[b_mixp], sembuf=b_mixp)
            yield

        def stage1b(t):
            cur[0] = upar(t)
            bk = proj_fm(1792, 128)
            cp(gq32, PS[bk][:, :], [PSB[bk]], [b_gq], eng="act")
            bk = proj_fm(1920, 128)
            cp(gk32, PS[bk][:, :], [PSB[bk]], [b_gk], eng="dve")
            bk = proj_fm(2448, 128)
            cp(gg32, PS[bk][:, :], [PSB[bk]], [b_gg], eng="act")
            for s in range(4):
                bk = proj_tm(2048, s)
                cp(gv16[:, s, :], PS[bk][:, 0:256], [PSB[bk]], [b_gv], eng="dve")
                act(Eb[:, s, :], PS[bk][:, 256:512], AF.Exp, [PSB[bk]], [b_Eb], scale=-1.0)
                cp(R[:, s, :], PS[bk][:, 256:512], [PSB[bk]], [b_R], eng="act")

        def gla(t):
            c0 = t * 512
            mm(PS[5][:, :], [(wg2[:, l * 128:(l + 1) * 128], gg32)], [b_wg2, b_gg], [PSB[5]])
            act(la, PS[5][:, :], AF.Exp, [PSB[5], b_misc], [b_la], scale=-1.0, bias=misc[:, M_NBG + l:M_NBG + l + 1])
            act(la, la, AF.Ln, [b_la], [b_la], bias=1.0)
            yield
            for s in range(4):
                S.op("dve", lambda e, s=s: e.tensor_tensor_scan(Bn[:, s * 128:(s + 1) * 128], ones32[:, :], la[:, s * 128:(s + 1) * 128],
                                                                0.0, ALU.mult, ALU.add), [b_la, b_ones], [b_Bn])
            act(eq, Bn, AF.Exp, [b_Bn], [b_eq], scale=-1.0 / 16.0)
            act(ek, Bn, AF.Exp, [b_Bn], [b_ek], scale=1.0 / 16.0)
            stt(qd16, gq32, 32 ** -0.5, eq, ALU.mult, ALU.mult, [b_gq, b_eq], [b_qd])
            tt(ki16, gk32, ek, ALU.mult, [b_gk, b_ek], [b_ki])
            for h in range(4):
                ts(Qm[:, :, h, :], qd16.rearrange("p (a b) -> p a b", a=4), c32[:, C_HM + h:C_HM + h + 1], None, ALU.mult, None,
                   [b_qd, b_c32], [b_Qm])
            yield
            act(Eb, Eb, AF.Ln, [b_Eb], [b_Eb], bias=1.0)
            act(Eb, Eb, AF.Exp, [b_Eb], [b_Eb], scale=-1.0)
            tt(R, R, Eb, ALU.mult, [b_R, b_Eb], [b_R])
            ggl = c32[:, C_GGLA + l * 256:C_GGLA + (l + 1) * 256]
            tt(R, R, ggl.unsqueeze(1).to_broadcast([128, 4, 256]), ALU.mult, [b_R, b_c32], [b_R])
            ps7b = PS[7][:, 0:256].bitcast(BF16)
            S.group("pe", [lambda e, s=s: e.transpose(ps7b[:, s * 128:(s + 1) * 128], ki16[:, s * 128:(s + 1) * 128], identb)
                           for s in range(4)], [b_ki, b_c16], [PSB[7]])
            cp(kitok, ps7b.rearrange("p (a b) -> p a b", a=4), [PSB[7]], [b_kitok], eng="act")
            yield
            ps1b = PS[1][:, 0:128].bitcast(BF16)
            for s in range(4):
                pz = s % 2
                attm, o32, osq, t1, y16, ssq4 = attm_l[pz], o32_l[pz], osq_l[pz], t1_l[pz], y16_l[pz], ssq4_l[pz]
                b_attm, b_o32, b_osq, b_t1, b_y16, b_ssq4 = (b_attm_l[pz], b_o32_l[pz], b_osq_l[pz], b_t1_l[pz], b_y16_l[pz],
                                                             b_ssq4_l[pz])
                mm(PS[5][:, :], [(ki16[:, s * 128:(s + 1) * 128], Qm[:, s, :, :])], [b_ki, b_Qm], [PSB[5]])
                mm(PS[7][:, 256:512], [(kitok[:, s, :], gv16[:, s, :])], [b_kitok, b_gv], [PSB[7]])
                tt(attm, PS[5][:, :], c16[:, K_GM:K_GM + 512], ALU.mult, [PSB[5], b_c16], [b_attm])
                tt(stmp, S32, PS[7][:, 256:512], ALU.add, [b_S32, PSB[7]], [b_stmp])
                yield
                fns = [lambda e, s=s: e.matmul(PS[6][:, 0:256], qd16[:, s * 128:(s + 1) * 128], Sbf, start=True, stop=False)]
                for h in range(4):
                    fns.append(lambda e, s=s, h=h, attm=attm: e.matmul(PS[6][:, h * 64:(h + 1) * 64], attm[:, h * 128:(h + 1) * 128],
                                                                       gv16[:, s, h * 64:(h + 1) * 64], start=False, stop=(h == 3)))
                S.group("pe", fns, [b_qd, b_Sbf, b_attm, b_gv], [PSB[6]])
                cp(o32, PS[6][:, 0:256], [PSB[6]], [b_o32], eng="act")
                stt(S32, stmp, eq[:, s * 128 + 127:s * 128 + 128], c32[:, C_BD:C_BD + 256], ALU.mult, ALU.mult,
                    [b_stmp, b_eq, b_c32], [b_S32])
                cp(Sbf, S32, [b_S32], [b_Sbf], eng="act")
                yield
                tt(osq, o32, o32, ALU.mult, [b_o32], [b_osq])
                S.op("dve", lambda e, osq=osq, ssq4=ssq4: e.tensor_reduce(ssq4, osq.rearrange("p (a b) -> p a b", a=4), AX.X, ALU.add),
                     [b_osq], [b_ssq4])
                act(ssq4, ssq4, AF.Ln, [b_ssq4], [b_ssq4], bias=EPS, scale=1.0 / 64.0)
                act(ssq4, ssq4, AF.Exp, [b_ssq4], [b_ssq4], scale=-0.5)
                tt(t1.rearrange("p (a b) -> p a b", a=4), o32.rearrange("p (a b) -> p a b", a=4),
                   ssq4.unsqueeze(2).to_broadcast([128, 4, 64]), ALU.mult, [b_o32, b_ssq4], [b_t1])
                tt(y16, t1, R[:, s, :], ALU.mult, [b_t1, b_R], [b_y16])
                yield
                S.group("pe", [lambda e, c=c, y16=y16: e.transpose(ps1b[:, c * 128:(c + 1) * 128], y16[:, c * 128:(c + 1) * 128], identb)
                               for c in range(2)], [b_y16, b_c16], [PSB[1]])
                cp(mixg[:, :, s * 128:(s + 1) * 128], ps1b.rearrange("p (a b) -> p a b", a=2), [PSB[1]], [b_mixg], eng="act")
                yield
            S.dma("sp", lambda e, c0=c0: e.dma_start(out=mixT_d[768:1024, c0:c0 + 512].rearrange("(j p) s -> p j s", p=128), in_=mixg),
                  reads=[b_mixg], sembuf=b_mixg)

        def round_robin(gens):
            gens = list(gens)
            while gens:
                for g in list(gens):
                    try:
                        next(g)
                    except StopIteration:
                        gens.remove(g)

        def chain(*gs):
            for g in gs:
                yield from g

        if l == 0:
            load_x(0)
        if DEEP:
            for _ in norm(0):
                pass
        for t in range(NT + 1):
            gens = []
            if t < NT:
                gens.append(projs(t) if DEEP else chain(norm(t), projs(t)))
            if t >= 1:
                gens.append(gla(t - 1))
            if DEEP and t + 1 < NT:
                gens.append(norm(t + 1))
            round_robin(gens)
            if t < NT:
                stage1b(t)

    def phase_B(l):
        kbytes = S_LEN * 2
        KT = [view(WA, 0, [S_LEN], BF16), view(WA, kbytes, [S_LEN], BF16)]
        VV = [view(WA, 2 * kbytes, [NKT, 128], BF16), view(WA, 3 * kbytes, [NKT, 128], BF16)]
        QT = [view(SCR, 2048 * i, [2, 512], BF16) for i in range(3)]
        Pt = [view(SCR, 6144 + 2048 * i, [1024], BF16) for i in range(4)]
        Psum = [view(SCR, 14336 + 2048 * i, [1024], BF16) for i in range(2)]
        Ptmp = view(SCR, 18432, [1024], BF16)
        r1 = view(SCR, 20480, [512])
        r2 = view(SCR, 22528, [512])
        A1 = view(SCR, 24576, [512])
        A2 = view(SCR, 26624, [512])
        ot = view(SCR, 28672, [512])
        osq = view(SCR, 30720, [512], BF16)
        rs = view(SCR, 31744, [512])
        yt = [view(SCR, 33792, [512], BF16), view(SCR, 34816, [512], BF16)]
        b_KT = [S.buf("KT0"), S.buf("KT1")]
        b_VV = [S.buf("VV0"), S.buf("VV1")]
        b_QT = [S.buf(f"QT{i}") for i in range(3)]
        b_P = [S.buf(f"P{i}") for i in range(4)]
        b_Psum = [S.buf("Psum0"), S.buf("Psum1")]
        b_Ptmp = S.buf("Ptmp")
        b_r1, b_r2, b_A1, b_A2, b_ot, b_osq, b_rs = (S.buf(n) for n in ("r1", "r2", "A1", "A2", "ot", "osq", "rs"))
        b_yt = [S.buf("yt0"), S.buf("yt1")]
        nlam = misc[:, M_NLAM + l:M_NLAM + l + 1]

        def load_head(h):
            S.dma("sp", lambda e: e.dma_start(out=KT[h % 2], in_=kT_d[h]), writes=[b_KT[h % 2]], sembuf=b_KT[h % 2])
            S.dma("sp", lambda e: e.dma_start(out=VV[h % 2], in_=v4_d[h]), writes=[b_VV[h % 2]], sembuf=b_VV[h % 2])

        jobs = [(h, qt) for h in range(4) for qt in range(NT)]

        def load_q(ji):
            h, qt = jobs[ji]
            S.dma("sp", lambda e: e.dma_start(out=QT[ji % 3], in_=qT_d[2 * h:2 * h + 2, :, qt * 512:(qt + 1) * 512].rearrange("m p s -> p m s")),
                  writes=[b_QT[ji % 3]], sembuf=b_QT[ji % 3])

        PEA = True
        njobs = len(jobs)
        steps = [(ji, kt) for ji, (h, qt) in enumerate(jobs) for kt in range(4 * qt + 4)]
        NS = len(steps)
        later = {}

        def defer(i, fn):
            later.setdefault(i, []).append(fn)

        def qk(i):
            ji, kt = steps[i]
            h, qt = jobs[ji]
            if kt == 2 and qt == 0 and h + 1 < 4:
                load_head(h + 1)
            if kt == 0 and ji + 2 < njobs:
                load_q(ji + 2)
            K_, Q_ = KT[h % 2], QT[ji % 3]
            for m in range(2):
                bank = 2 * (kt % 2) + m
                mm(PS[bank][:, :], [(K_[:, kt * 128:(kt + 1) * 128], Q_[:, m, :])], [b_KT[h % 2], b_QT[ji % 3]], [PSB[bank]], attach=PEA)

        def expo(i):
            ji, kt = steps[i]
            h, qt = jobs[ji]
            nk = 4 * qt + 4
            b0 = 2 * (kt % 2)
            P_ = Pt[kt % 4]
            bP = b_P[kt % 4]
            act(P_, PSALL[:, b0 * 512:(b0 + 2) * 512], AF.Exp, [PSB[b0], PSB[b0 + 1]], [bP])
            if kt >= 4 * qt:
                r = kt - 4 * qt
                msk = c16[:, K_AM + r * 512:K_AM + (r + 1) * 512].unsqueeze(1).to_broadcast([128, 2, 512])
                P3 = P_.rearrange("p (a b) -> p a b", a=2)
                tt(P3, P3, msk, ALU.mult, [bP, b_c16], [bP])
            g = kt // 4
            if kt % 4 == 1:
                tt(Psum[g % 2], Pt[(kt - 1) % 4], P_, ALU.add, [b_P[(kt - 1) % 4], bP], [b_Psum[g % 2]])
            if kt % 4 == 3 and kt != nk - 1:
                tt(Ptmp, Pt[(kt - 1) % 4], P_, ALU.add, [b_P[(kt - 1) % 4], bP], [b_Ptmp])
                tt(Psum[g % 2], Psum[g % 2], Ptmp, ALU.add, [b_Ptmp, b_Psum[g % 2]], [b_Psum[g % 2]])

        def den_mm(rhs_ap, rbufs, first, last):
            for m in range(2):
                S.group("pe", [lambda e, m=m, first=first, last=last: e.matmul(
                    PS[6 + m][:, :], ones16[:, :], rhs_ap[:, m * 512:(m + 1) * 512], start=first, stop=last)],
                    [b_ones] + rbufs, [PSB[6 + m]], attach=PEA)

        def av(i):
            ji, kt = steps[i]
            h, qt = jobs[ji]
            nk = 4 * qt + 4
            V_ = VV[h % 2]
            P_ = Pt[kt % 4]
            for m in range(2):
                S.group("pe", [lambda e, m=m: e.matmul(
                    PS[4 + m][:, :], V_[:, kt, :], P_[:, m * 512:(m + 1) * 512], start=(kt == 0), stop=(kt == nk - 1))],
                    [b_VV[h % 2], b_P[kt % 4]], [PSB[4 + m]], attach=PEA)
            if kt % 4 == 1 and kt >= 5:
                g = (kt - 5) // 4
                den_mm(Psum[g % 2], [b_Psum[g % 2]], g == 0, False)
            if kt == nk - 1:
                g = nk // 4 - 1
                den_mm(Psum[g % 2], [b_Psum[g % 2]], g == 0, False)
                den_mm(Pt[(kt - 1) % 4], [b_P[(kt - 1) % 4]], False, False)
                den_mm(P_, [b_P[kt % 4]], False, True)
                cp(A1, PS[4][:, :], [PSB[4]], [b_A1], eng="dve")
                act(r1, PS[6][:, :], AF.Ln, [PSB[6]], [b_r1])
                cp(A2, PS[5][:, :], [PSB[5]], [b_A2], eng="dve")
                act(r2, PS[7][:, :], AF.Ln, [PSB[7]], [b_r2])

                def e_a():
                    act(r1, r1, AF.Exp, [b_r1], [b_r1], scale=-1.0)
                    act(r2, r2, AF.Exp, [b_r2], [b_r2], scale=-1.0)

                def e_b():
                    tt(A1, A1, r1, ALU.mult, [b_A1, b_r1], [b_A1])
                    tt(A2, A2, r2, ALU.mult, [b_A2, b_r2], [b_A2])
                    stt(ot, A2, nlam, A1, ALU.mult, ALU.add, [b_A2, b_A1, b_misc], [b_ot])

                def e_c():
                    act(osq, ot, AF.Square, [b_ot], [b_osq])

                def e_d():
                    mm(PS[0][:, :], [(onesV[:, :], osq)], [b_osq, b_ones], [PSB[0]])
                    act(rs, PS[0][:, :], AF.Ln, [PSB[0]], [b_rs], bias=EPS)
                    act(rs, rs, AF.Exp, [b_rs], [b_rs], scale=-0.5)

                def e_e(ji=ji, h=h, qt=qt):
                    y_ = yt[ji % 2]
                    stt(y_, ot, misc[:, M_GD + 4 * l + h:M_GD + 4 * l + h + 1], rs, ALU.mult, ALU.mult, [b_ot, b_rs, b_misc], [b_yt[ji % 2]])
                    S.dma("sp", lambda e: e.dma_start(out=mixT_d[256 + h * 128:256 + (h + 1) * 128, qt * 512:(qt + 1) * 512], in_=y_),
                          reads=[b_yt[ji % 2]], sembuf=b_yt[ji % 2])
                for d_, f_ in enumerate((e_a, e_b, e_c, e_d, e_e)):
                    defer(i + 2 + d_, f_)

        load_head(0)
        load_q(0)
        if njobs > 1:
            load_q(1)
        qk(0)
        for i in range(NS):
            if i + 1 < NS:
                qk(i + 1)
            if i >= 1:
                av(i - 1)
            expo(i)
            for f_ in later.pop(i, []):
                f_()
        av(NS - 1)
        for k_ in sorted(later):
            for f_ in later[k_]:
                f_()

    def phase_C(l):
        g2 = c32[:, C_G2 + l * 8:C_G2 + (l + 1) * 8]
        hA = [view(SCR, 0, [8, 512]), view(SCR, 16384, [8, 512])]
        mx = [view(SCR, 32768, [8, 512], BF16), view(SCR, 40960, [8, 512], BF16)]
        sq = view(WB, 16384, [8, 512], BF16)
        rstd = view(SCR, 51200, [512])
        zT = view(SCR, 53248, [8, 512], BF16)
        b_hA = [S.buf("ChA0"), S.buf("ChA1")]
        b_mx = [S.buf("Cmx0"), S.buf("Cmx1")]
        b_sq = [S.buf(f"Csq{c}") for c in range(8)]
        b_rstd, b_zT = S.buf("Crstd"), S.buf("CzT")

        def load(t):
            i = t % 2
            S.dma("sp", lambda e: e.dma_start(out=hA[i], in_=hT_d[:, t * 512:(t + 1) * 512].rearrange("(k p) s -> p k s", p=128)),
                  writes=[b_hA[i]], sembuf=b_hA[i])
            S.dma("sp", lambda e: e.dma_start(out=mx[i], in_=mixT_d[:, t * 512:(t + 1) * 512].rearrange("(k p) s -> p k s", p=128)),
                  writes=[b_mx[i]], sembuf=b_mx[i])

        load(0)
        for t in range(NT):
            i = t % 2
            if t + 1 < NT:
                load(t + 1)
            for c in range(8):
                bank = c % 4
                mm(PS[bank][:, :], [(WoutV[:, k, c * 128:(c + 1) * 128], mx[i][:, k, :]) for k in range(8)],
                   [b_wout, b_mx[i]], [PSB[bank]])
                tt(hA[i][:, c, :], hA[i][:, c, :], PS[bank][:, :], ALU.add, [PSB[bank], b_hA[i]], [b_hA[i]])
                act(sq[:, c, :], hA[i][:, c, :], AF.Square, [b_hA[i]], [b_sq[c]])
            for c in range(8):
                S.group("pe", [lambda e, c=c: e.matmul(PS[4][:, :], onesD[:, :], sq[:, c, :], start=(c == 0), stop=(c == 7))],
                        [b_sq[c], b_ones], [PSB[4]])
            act(rstd, PS[4][:, :], AF.Ln, [PSB[4]], [b_rstd], bias=EPS)
            act(rstd, rstd, AF.Exp, [b_rstd], [b_rstd], scale=-0.5)
            for c in range(8):
                stt(zT[:, c, :], hA[i][:, c, :], g2[:, c:c + 1], rstd, ALU.mult, ALU.mult, [b_hA[i], b_rstd, b_c32], [b_zT])
            S.dma("sp", lambda e, i=i, t=t: e.dma_start(out=hT_d[:, t * 512:(t + 1) * 512].rearrange("(k p) s -> p k s", p=128), in_=hA[i]),
                  reads=[b_hA[i]], sembuf=b_hA[i])
            S.dma("sp", lambda e, t=t: e.dma_start(out=zT_d[:, t * 512:(t + 1) * 512].rearrange("(k p) s -> p k s", p=128), in_=zT),
                  reads=[b_zT], sembuf=b_zT)

    def phase_D(l, hf, final):
        W1 = W1V[hf]
        W2 = W2V[hf]
        hA = [view(SCR, 0, [8, 512]), view(SCR, 16384, [8, 512])]
        zT = view(SCR, 32768, [8, 512], BF16)
        hid = view(SCR, 40960, [16, 512], BF16)
        rt = [view(SCR, 57344, [512]), view(SCR, 59392, [512])]
        sq = view(SCR, 61440, [2, 512], BF16)
        rstd = view(SCR, 63488, [512])
        ost = [view(WA, 0, [1024]), view(WA, 4096, [1024])]
        b_hA = [S.buf("DhA0"), S.buf("DhA1")]
        b_zT = S.buf("DzT")
        b_hid = [S.buf(f"hid{f}") for f in range(16)]
        b_rt = [S.buf("rt0"), S.buf("rt1")]
        b_sq = [S.buf("Dsq0"), S.buf("Dsq1")]
        b_rstd = S.buf("Drstd")
        b_ost = [S.buf("ost0"), S.buf("ost1")]
        gf = c32[:, C_GF:C_GF + 8]

        def load_h(t):
            i = t % 2
            S.dma("sp", lambda e: e.dma_start(out=hA[i], in_=hT_d[:, t * 512:(t + 1) * 512].rearrange("(k p) s -> p k s", p=128)),
                  writes=[b_hA[i]], sembuf=b_hA[i])

        def load_z(t):
            S.dma("sp", lambda e: e.dma_start(out=zT, in_=zT_d[:, t * 512:(t + 1) * 512].rearrange("(k p) s -> p k s", p=128)),
                  writes=[b_zT], sembuf=b_zT)

        load_h(0)
        load_z(0)
        oi = 0
        for t in range(NT):
            i = t % 2
            if t + 1 < NT:
                load_h(t + 1)
            for f in range(16):
                bank = f % 4
                mm(PS[bank][:, :], [(W1[:, k, f * 128:(f + 1) * 128], zT[:, k, :]) for k in range(8)], [b_w1[hf], b_zT], [PSB[bank]])
                r_ = rt[f % 2]
                act(r_, PS[bank][:, :], AF.Relu, [PSB[bank]], [b_rt[f % 2]])
                tt(hid[:, f, :], r_, PS[bank][:, :], ALU.mult, [b_rt[f % 2], PSB[bank]], [b_hid[f]])
            if t + 1 < NT:
                load_z(t + 1)
            for c in range(8):
                bank = 4 + c % 4
                mm(PS[bank][:, :], [(W2[:, f, c * 128:(c + 1) * 128], hid[:, f, :]) for f in range(16)], [b_w2[hf]] + b_hid, [PSB[bank]])
                tt(hA[i][:, c, :], hA[i][:, c, :], PS[bank][:, :], ALU.add, [PSB[bank], b_hA[i]], [b_hA[i]])
            if not final:
                S.dma("sp", lambda e, i=i, t=t: e.dma_start(out=hT_d[:, t * 512:(t + 1) * 512].rearrange("(k p) s -> p k s", p=128), in_=hA[i]),
                      reads=[b_hA[i]], sembuf=b_hA[i])
            else:
                for c in range(8):
                    act(sq[:, c % 2, :], hA[i][:, c, :], AF.Square, [b_hA[i]], [b_sq[c % 2]])
                    S.group("pe", [lambda e, c=c: e.matmul(PS[0][:, :], onesD[:, :], sq[:, c % 2, :], start=(c == 0), stop=(c == 7))],
                            [b_sq[c % 2], b_ones], [PSB[0]])
                act(rstd, PS[0][:, :], AF.Ln, [PSB[0]], [b_rstd], bias=EPS)
                act(rstd, rstd, AF.Exp, [b_rstd], [b_rstd], scale=-0.5)
                for c in range(8):
                    stt(hA[i][:, c, :], hA[i][:, c, :], gf[:, c:c + 1], rstd, ALU.mult, ALU.mult, [b_hA[i], b_rstd, b_c32], [b_hA[i]])
                for s in range(4):
                    o_ = ost[oi % 2]
                    bo = b_ost[oi % 2]
                    for half in range(2):
                        bank = 1 + half
                        S.group("pe", [lambda e, kk=kk, half=half, bank=bank, s=s, i=i: e.transpose(
                            PS[bank][:, kk * 128:(kk + 1) * 128], hA[i][:, half * 4 + kk, s * 128:(s + 1) * 128], ident32) for kk in range(4)],
                            [b_hA[i], b_c32], [PSB[bank]])
                        cp(o_[:, half * 512:(half + 1) * 512], PS[bank][:, :], [PSB[bank]], [bo], eng="act" if half == 0 else "dve")
                    S.dma("sp", lambda e, o_=o_, t=t, s=s: e.dma_start(out=out_d[t * 512 + s * 128:t * 512 + (s + 1) * 128, :], in_=o_),
                          reads=[bo], sembuf=bo)
                    oi += 1

    def done(tag):
        return stop_after is not None and stop_after == tag

    S.barrier()
    finished = False
    for l in range(n_layers):
        if l > 0:
            load_wout(l)
        phase_A(l)
        S.barrier()
        if done(f"A{l}"):
            break
        phase_B(l)
        S.barrier()
        if done(f"B{l}"):
            break
        load_w12(l, 0)
        phase_C(l)
        S.barrier()
        if done(f"C{l}"):
            break
        load_w12(l, 1)
        phase_D(l, 0, False)
        S.barrier()
        if l + 1 < n_layers:
            load_win(l + 1)
        phase_D(l, 1, l == n_layers - 1)
        S.barrier()
    S.finish()
    return nc, S


def _host_inputs(inp, core_x):
    c32, c16 = _const_tables()
    c32 = _fill_params(c32, inp)
    wg2 = np.ascontiguousarray(np.asarray(inp["gla_w_gate2"], np.float32).transpose(1, 0, 2).reshape(16, L * 128))
    return {
        "x": np.ascontiguousarray(core_x, dtype=np.float32),
        "w_in": np.asarray(inp["w_in"], np.float32),
        "w_out": np.asarray(inp["w_out"], np.float32),
        "w_mlp1": np.asarray(inp["w_mlp1"], np.float32),
        "w_mlp2": np.asarray(inp["w_mlp2"], np.float32),
        "wg2": wg2,
        "pool_w": np.asarray(inp["pool_w"], np.float32),
        "c32": c32,
        "c16": c16,
    }


def kernel(**inputs):
    x = np.asarray(inputs["x"], np.float32)
    B, S_LEN, _ = x.shape
    nc, _ = build_nc(S_LEN)
    base = _host_inputs(inputs, x[0])
    in_maps = []
    for b in range(B):
        m = dict(base)
        m["x"] = np.ascontiguousarray(x[b])
        in_maps.append(m)
    res = run_bass_kernel_spmd(nc, in_maps, core_ids=list(range(B)))
    return np.stack([np.asarray(r["out"], np.float32) for r in res.results], axis=0)
```

```python
import math
import numpy as np
import concourse.bass as bass
import concourse.mybir as mybir
from concourse.bass_utils import run_bass_kernel_spmd

F32 = mybir.dt.float32
BF16 = mybir.dt.bfloat16
ALU = mybir.AluOpType
AF = mybir.ActivationFunctionType
AX = mybir.AxisListType

L = 2
D = 1024
DIN = 2576
DFF = 4096
EPS = 1e-6
SEM_ROT = 24000
import os
SKIP = set(os.environ.get('K_SKIP', '').split(','))
ATTACH = os.environ.get('K_ATTACH', '1') == '1'


class Buf:
    __slots__ = ("name", "w", "r", "dsem", "dcnt", "excl")

    def __init__(self, name):
        self.name = name
        self.excl = False
        self.w = []
        self.r = []
        self.dsem = None
        self.dcnt = 0


class Eng:
    def __init__(self, name):
        self.name = name
        self.sem = None
        self.cnt = 0
        self.seen = {}
        self.recs = []


def _merge(evts):
    d = {}
    for (s, v) in evts:
        k = id(s)
        if k not in d or d[k][1] < v:
            d[k] = (s, v)
    return list(d.values())


class Sched:
    def __init__(self, nc):
        self.nc = nc
        self.engs = {n: Eng(n) for n in ("pe", "act", "dve", "pool", "sp")}
        self.nsem = 0
        self.ninst = 0
        self.dbufs = []
        self.cache = {}
        self.engsems = set()

    def new_sem(self, name):
        self.nsem += 1
        sm = self.nc.alloc_semaphore(name=f"{name}_{self.nsem}")
        if name.startswith("p_"):
            self.engsems.add(id(sm))
        return sm

    def buf(self, name):
        if name not in self.cache:
            self.cache[name] = Buf(name)
        return self.cache[name]

    def _deps(self, reads, writes):
        ev = []
        for b in reads:
            ev.extend(b.w)
        for b in writes:
            ev.extend(b.w)
            ev.extend(b.r)
        return _merge(ev)

    def _need(self, eng, evts):
        waits = []
        for (s, v) in evts:
            k = id(s)
            if k in eng.seen and eng.seen[k][1] >= v:
                continue
            eng.seen[k] = (s, v)
            waits.append((s, v))
        return waits

    def _commit(self, ev, reads, writes):
        for b in writes:
            b.w = [ev]
            b.r = []
        for b in reads:
            if b in writes:
                continue
            b.r = [e for e in b.r if e[0] is not ev[0]] + [ev]

    def _eng_ev(self, eng):
        if eng.sem is None or eng.cnt >= SEM_ROT:
            eng.sem = self.new_sem("p_" + eng.name)
            eng.cnt = 0
        eng.cnt += 1
        return (eng.sem, eng.cnt)

    def op(self, engname, fn, reads=(), writes=()):
        return self.group(engname, [fn], reads, writes)

    def group(self, engname, fns, reads=(), writes=(), attach=None):
        eng = self.engs[engname]
        if attach is None:
            attach = ATTACH
        xr = [b for b in reads if b.excl]
        if xr:
            reads = [b for b in reads if not b.excl]
            writes = list(writes) + [b for b in xr if b not in writes]
        rdeps = self._deps(reads, ())
        waits = self._need(eng, self._deps(reads, writes))
        ev = self._eng_ev(eng)
        aw = None
        if attach and ATTACH:
            rd = {id(s_): v_ for (s_, v_) in rdeps}
            for w_ in reversed(waits):
                if id(w_[0]) not in self.engsems:
                    continue
                if engname == "pe" and id(w_[0]) in rd and rd[id(w_[0])] >= w_[1]:
                    continue
                aw = w_
                break
            if aw is not None:
                waits = [w_ for w_ in waits if w_ is not aw]
        eng.recs.append((waits, list(fns), (ev[0], 1, False), aw))
        self._commit(ev, reads, writes)
        self.ninst += len(fns)
        return ev

    def dma(self, qname, fns, reads=(), writes=(), sembuf=None):
        if not isinstance(fns, (list, tuple)):
            fns = [fns]
        eng = self.engs[qname]
        sb = sembuf
        if sb.dsem is None or sb.dcnt + 16 * len(fns) >= SEM_ROT:
            sb.dsem = self.new_sem("d_" + sb.name)
            sb.dcnt = 0
            self.dbufs.append(sb)
        waits = self._need(eng, self._deps(reads, writes))
        sb.dcnt += 16 * len(fns)
        ev = (sb.dsem, sb.dcnt)
        eng.recs.append((waits, list(fns), (sb.dsem, 16, True)))
        self._commit(ev, reads, writes)
        self.ninst += len(fns)
        return ev

    def barrier(self):
        ev = []
        for e in self.engs.values():
            if e.sem is not None:
                ev.append((e.sem, e.cnt))
        seen_ids = set()
        for b in self.dbufs:
            if b.dsem is not None and id(b.dsem) not in seen_ids:
                seen_ids.add(id(b.dsem))
                ev.append((b.dsem, b.dcnt))
        ev = _merge(ev)
        for e in self.engs.values():
            waits = self._need(e, ev)
            if waits:
                e.recs.append((waits, [], None))

    def finish(self):
        self.barrier()
        nc = self.nc

        def replay(eng):
            def run(e):
                for rec in eng.recs:
                    waits, fns, inc = rec[0], rec[1], rec[2]
                    aw = rec[3] if len(rec) > 3 else None
                    for (s, v) in waits:
                        e.wait_ge(s, v)
                    last = None
                    for f in fns:
                        last = f(e)
                        if aw is not None:
                            last._wait_ge(aw[0], aw[1])
                            aw = None
                        if inc is not None and inc[2]:
                            last.then_inc(inc[0], inc[1])
                    if inc is not None and not inc[2] and last is not None:
                        last.then_inc(inc[0], inc[1])
            return run

        with nc.Block() as block:
            block.tensor(replay(self.engs["pe"]))
            block.scalar(replay(self.engs["act"]))
            block.vector(replay(self.engs["dve"]))
            block.gpsimd(replay(self.engs["pool"]))
            block.sync(replay(self.engs["sp"]))


C_ID = 0
C_BD = 128
C_HM = 384
C_CORR = 388
C_G1 = 420
C_G2 = C_G1 + L * 8
C_GF = C_G2 + L * 8
C_PSC = C_GF + 8
C_GD = C_PSC + L * 2
C_BG = C_GD + L * 4
C_GGLA = C_BG + L
C_LQ = C_GGLA + L * 256
NC32 = C_LQ + L * 256
K_AM = 0
K_GM = 2048
K_IDB = 2560
NC16 = 2688

POOL_WINS = (2, 4, 8, 16)


def _const_tables():
    c32 = np.zeros((128, NC32), np.float32)
    c32[:, C_ID:C_ID + 128] = np.eye(128, dtype=np.float32)
    p = np.arange(128)
    c = np.arange(256)
    c32[:, C_BD:C_BD + 256] = (p[:, None] // 32 == c[None, :] // 64).astype(np.float32)
    c32[:, C_HM:C_HM + 4] = (p[:, None] // 32 == np.arange(4)[None, :]).astype(np.float32)
    corr = np.zeros((128, 2, 16), np.float32)
    for j in range(2):
        for half in range(2):
            w = POOL_WINS[2 * j + half]
            i = np.arange(16)
            corr[half * 64:(half + 1) * 64, j, :] = w / np.minimum(i + 1, w)
    c32[:, C_CORR:C_CORR + 32] = corr.reshape(128, 32)
    c16 = np.zeros((128, NC16), np.float32)
    jj = np.arange(128)
    ii = np.arange(512)
    for r in range(4):
        c16[:, K_AM + r * 512:K_AM + (r + 1) * 512] = (
            (2 * r + jj[:, None] // 64) <= (ii[None, :] // 64)).astype(np.float32)
    gm = (jj[:, None] <= np.arange(128)[None, :]).astype(np.float32)
    c16[:, K_GM:K_GM + 512] = np.tile(gm, (1, 4))
    c16[:, K_IDB:K_IDB + 128] = np.eye(128, dtype=np.float32)
    return c32, c16


def _fill_params(c32, inp):
    def cols(v, n):
        return np.ascontiguousarray(np.asarray(v, np.float32).reshape(n, 128).T)
    for l in range(L):
        c32[:, C_G1 + l * 8:C_G1 + (l + 1) * 8] = cols(inp["norm1_g"][l], 8)
        c32[:, C_G2 + l * 8:C_G2 + (l + 1) * 8] = cols(inp["norm2_g"][l], 8)
        c32[:, C_PSC + l * 2:C_PSC + (l + 1) * 2] = cols(inp["pool_scale"][l], 2)
        c32[:, C_GD + l * 4:C_GD + (l + 1) * 4] = cols(inp["diff_norm_g"][l], 4)
        c32[:, C_BG + l:C_BG + l + 1] = cols(inp["gla_b_gate"][l], 1)
        c32[:, C_GGLA + l * 256:C_GGLA + (l + 1) * 256] = np.asarray(inp["gla_norm_g"][l], np.float32)[None, :]
        for i, nm in enumerate(("diff_lq1", "diff_lk1", "diff_lq2", "diff_lk2")):
            c32[:, C_LQ + l * 256 + i * 64:C_LQ + l * 256 + (i + 1) * 64] = np.asarray(inp[nm][l], np.float32)[None, :]
    c32[:, C_GF:C_GF + 8] = cols(inp["final_norm_g"], 8)
    return c32


def build_nc(S_LEN, debug=False, n_layers=L, stop_after=None):
    NT = S_LEN // 512
    NKT = S_LEN // 128
    nc = bass.Bass("TRN2", target_bir_lowering=False)
    S = Sched(nc)

    def din(name, shape, dt=F32):
        return nc.dram_tensor(name, list(shape), dt, kind="ExternalInput").ap()

    x_d = din("x", [S_LEN, D])
    win_d = din("w_in", [L, D, DIN])
    wout_d = din("w_out", [L, D, D])
    w1_d = din("w_mlp1", [L, D, DFF])
    w2_d = din("w_mlp2", [L, DFF, D])
    wg2_d = din("wg2", [16, L * 128])
    poolw_d = din("pool_w", [L, 4, 64, 64])
    c32_d = din("c32", [128, NC32])
    c16_d = din("c16", [128, NC16])
    out_d = nc.dram_tensor("out", [S_LEN, D], F32, kind="ExternalOutput").ap()
    skind = "ExternalOutput" if debug else "Internal"

    def dscr(name, shape, dt):
        return nc.dram_tensor(name, list(shape), dt, kind=skind).ap()

    hT_d = dscr("hT", [D, S_LEN], F32)
    qT_d = dscr("qT", [8, 128, S_LEN], BF16)
    kT_d = dscr("kT", [4, 128, S_LEN], BF16)
    v4_d = dscr("v4", [4, 128, NKT, 128], BF16)
    mixT_d = dscr("mixT", [D, S_LEN], BF16)
    zT_d = dscr("zT", [D, S_LEN], BF16)

    def sbt(name, cols, dt=F32):
        return nc.sbuf_tensor(name, [128, cols], dt).__enter__()

    WA = sbt("WA", 16384)
    WB = sbt("WB", 16384)
    SCR = sbt("SCR", 16384)
    c32 = sbt("c32s", NC32)
    c16 = sbt("c16s", NC16, BF16)
    wg2 = sbt("wg2s", L * 128)
    poolW = sbt("poolW", L * 256, BF16)
    misc = sbt("misc", 64)
    ones16 = sbt("ones16", 128, BF16)
    onesD = sbt("onesD", 128, BF16)
    onesV = sbt("onesV", 128, BF16)
    ones32 = sbt("ones32", 128)
    PSALL = nc.psum_tensor("psall", [128, 4096], F32).__enter__()
    PS = [PSALL[:, i * 512:(i + 1) * 512] for i in range(8)]
    PSB = [S.buf(f"ps{i}") for i in range(8)]
    for b_ in PSB:
        b_.excl = True

    def view(t, off, shape, dt=F32):
        n = 1
        for s_ in shape:
            n *= s_
        nbytes = n * (4 if dt == F32 else 2)
        assert off % 4 == 0 and nbytes % 4 == 0 and off + nbytes <= 65536, (off, nbytes)
        ap = t[:, off // 4:(off + nbytes) // 4]
        if dt != F32:
            ap = ap.bitcast(dt)
        if len(shape) == 2:
            ap = ap.rearrange("p (a b) -> p a b", a=shape[0])
        elif len(shape) == 3:
            ap = ap.rearrange("p (a b c) -> p a b c", a=shape[0], b=shape[1])
        return ap

    def act(out, in_, func, reads, writes, **kw):
        S.op("act", lambda e: e.activation(out, in_, func, **kw), reads, writes)

    def tt(out, a, b, op, reads, writes, eng="dve"):
        S.op(eng, lambda e: e.tensor_tensor(out, a, b, op), reads, writes)

    def stt(out, in0, scalar, in1, op0, op1, reads, writes, eng="dve"):
        S.op(eng, lambda e: e.scalar_tensor_tensor(out, in0, scalar, in1, op0, op1), reads, writes)

    def ts(out, in0, s1, s2, op0, op1, reads, writes, eng="dve"):
        if op1 is None:
            S.op(eng, lambda e: e.tensor_scalar(out, in0, s1, None, op0), reads, writes)
        else:
            S.op(eng, lambda e: e.tensor_scalar(out, in0, s1, s2, op0, op1), reads, writes)

    def cp(out, in_, reads, writes, eng="dve"):
        if eng == "act":
            S.op("act", lambda e: e.activation(out, in_, AF.Copy), reads, writes)
        else:
            S.op(eng, lambda e: e.tensor_copy(out, in_), reads, writes)

    def mm(out, pairs, reads, writes, start=True, attach=False):
        n = len(pairs)
        fns = []
        for i, (lt, rh) in enumerate(pairs):
            fns.append(lambda e, lt=lt, rh=rh, i=i: e.matmul(out, lt, rh, start=(start and i == 0), stop=(i == n - 1)))
        S.group("pe", fns, reads, writes, attach=attach)

    def rsqrt_inplace(t_ap, buf, scale=1.0):
        act(t_ap, t_ap, AF.Ln, [buf], [buf], bias=EPS, scale=scale)
        act(t_ap, t_ap, AF.Exp, [buf], [buf], scale=-0.5)

    b_c32, b_c16, b_wg2, b_poolW, b_misc, b_ones = (S.buf(n) for n in ("c32", "c16", "wg2", "poolW", "misc", "ones"))
    S.dma("sp", lambda e: e.dma_start(out=c32[:], in_=c32_d), writes=[b_c32], sembuf=b_c32)
    S.op("dve", lambda e: e.memset(wg2[:], 0.0), writes=[b_wg2])
    S.dma("sp", lambda e: e.dma_start(out=wg2[112:128, :], in_=wg2_d), writes=[b_wg2], sembuf=b_wg2)
    S.dma("pool", lambda e: e.dma_start(out=c16[:], in_=c16_d), writes=[b_c16], sembuf=b_c16)
    S.op("dve", lambda e: e.memset(poolW[:], 0.0), writes=[b_poolW])
    pw_fns = []
    for l in range(L):
        for g in range(4):
            j, half = g // 2, g % 2
            pw_fns.append(lambda e, l=l, g=g, j=j, half=half: e.dma_start(
                out=poolW[half * 64:(half + 1) * 64, l * 256 + j * 128 + half * 64: l * 256 + j * 128 + half * 64 + 64],
                in_=poolw_d[l, g]))
    S.dma("pool", pw_fns, writes=[b_poolW], sembuf=b_poolW)
    S.op("dve", lambda e: e.memset(ones16[:], 1.0), writes=[b_ones])
    S.op("dve", lambda e: e.memset(onesD[:], 1.0 / 1024.0), writes=[b_ones])
    S.op("dve", lambda e: e.memset(onesV[:], 1.0 / 128.0), writes=[b_ones])
    S.op("dve", lambda e: e.memset(ones32[:], 1.0), writes=[b_ones])
    ident32 = c32[:, C_ID:C_ID + 128]
    identb = c16[:, K_IDB:K_IDB + 128]
    M_NLAM, M_NBG, M_GD = 0, 4, 8
    for l in range(L):
        lam_init = 0.8 - 0.6 * math.exp(-0.3 * l)
        lq = c32[:, C_LQ + l * 256:C_LQ + (l + 1) * 256]
        tmp = misc[:, 32:34]
        prod = view(SCR, 0, [256])
        b_tmp = S.buf("lamtmp")
        tt(prod[:, 0:64], lq[:, 0:64], lq[:, 64:128], ALU.mult, [b_c32], [b_tmp])
        tt(prod[:, 64:128], lq[:, 128:192], lq[:, 192:256], ALU.mult, [b_c32], [b_tmp])
        S.op("dve", lambda e, prod=prod, tmp=tmp: e.tensor_reduce(
            tmp, prod[:, 0:128].rearrange("p (a b) -> p a b", a=2), AX.X, ALU.add), [b_tmp], [b_misc])
        act(tmp, tmp, AF.Exp, [b_misc], [b_misc])
        stt(misc[:, M_NLAM + l:M_NLAM + l + 1], tmp[:, 1:2], -lam_init, tmp[:, 0:1], ALU.add, ALU.subtract, [b_misc], [b_misc])
        ts(misc[:, M_NBG + l:M_NBG + l + 1], c32[:, C_BG + l:C_BG + l + 1], -1.0, None, ALU.mult, None, [b_c32], [b_misc])
        ts(misc[:, M_GD + 4 * l:M_GD + 4 * l + 4], c32[:, C_GD + 4 * l:C_GD + 4 * l + 4], 1.0 - lam_init, None, ALU.mult, None,
           [b_c32], [b_misc])

    b_win, b_wout = S.buf("win"), S.buf("wout")
    b_w1 = [S.buf("w1a"), S.buf("w1b")]
    b_w2 = [S.buf("w2a"), S.buf("w2b")]
    WinV = view(WA, 0, [8, DIN], BF16)
    WoutV = view(WB, 0, [8, D], BF16)
    W1V = [view(WA, 0, [8, 2048], BF16), view(WB, 0, [8, 2048], BF16)]
    W2V = [view(WA, 32768, [16, D], BF16), view(WB, 32768, [16, D], BF16)]

    def load_win(l):
        S.dma("pool", [lambda e, kc=kc: e.dma_start(out=WinV[:, kc, :], in_=win_d[l, kc * 128:(kc + 1) * 128, :])
                       for kc in range(8)], writes=[b_win], sembuf=b_win)

    def load_wout(l):
        S.dma("pool", [lambda e, kc=kc: e.dma_start(out=WoutV[:, kc, :], in_=wout_d[l, kc * 128:(kc + 1) * 128, :])
                       for kc in range(8)], writes=[b_wout], sembuf=b_wout)

    def load_w12(l, hf):
        S.dma("pool", [lambda e, kc=kc: e.dma_start(out=W1V[hf][:, kc, :],
                                                    in_=w1_d[l, kc * 128:(kc + 1) * 128, hf * 2048:(hf + 1) * 2048])
                       for kc in range(8)], writes=[b_w1[hf]], sembuf=b_w1[hf])
        S.dma("pool", [lambda e, f=f: e.dma_start(out=W2V[hf][:, f, :],
                                                  in_=w2_d[l, (hf * 16 + f) * 128:(hf * 16 + f + 1) * 128, :])
                       for f in range(16)], writes=[b_w2[hf]], sembuf=b_w2[hf])

    load_win(0)
    load_wout(0)

    def phase_A(l):
        g1 = c32[:, C_G1 + l * 8:C_G1 + (l + 1) * 8]
        hA = view(SCR, 0, [8, 512])
        uTs = [view(SCR, 16384, [8, 512], BF16), view(WA, 43008, [8, 512], BF16)]
        xs = view(SCR, 24576, [2, 1024])
        sq = view(SCR, 32768, [2, 512], BF16)
        rstd = view(SCR, 34816, [512])
        pE = view(SCR, 36864, [2, 528])
        pX = view(SCR, 41088, [2, 528])
        pY = view(SCR, 45312, [2, 528])
        pM = view(SCR, 49536, [2, 512])
        pL = view(SCR, 53632, [2, 512], BF16)
        mixp = view(SCR, 55680, [2, 512], BF16)
        mixg = view(SCR, 57728, [2, 512], BF16)
        gg32 = view(SCR, 59776, [512])
        S32 = view(SCR, 61824, [256])
        Sbf = view(SCR, 62848, [256], BF16)
        stmp = view(SCR, 63360, [256])
        ssq4 = view(SCR, 64384, [4])
        qst = view(WB, 16384, [8, 512], BF16)
        kst = view(WB, 24576, [4, 512], BF16)
        vst = view(WB, 28672, [4, 512], BF16)
        gq32 = view(WB, 32768, [512])
        gk32 = view(WB, 34816, [512])
        la = view(WB, 36864, [512])
        Bn = view(WB, 38912, [512])
        eq = view(WB, 40960, [512])
        ek = view(WB, 43008, [512])
        qd16 = view(WB, 45056, [512], BF16)
        ki16 = view(WB, 46080, [512], BF16)
        Qm = view(WB, 47104, [4, 4, 128], BF16)
        kitok = view(WB, 51200, [4, 128], BF16)
        gv16 = view(WB, 52224, [4, 256], BF16)
        R = view(WB, 54272, [4, 256])
        Eb = view(WB, 58368, [4, 256])
        attm = view(WB, 62464, [512], BF16)
        o32 = view(WB, 63488, [256])
        osq = view(WB, 64512, [256])
        t1 = view(WA, 41216, [256])
        y16 = view(WA, 42240, [256], BF16)
        attm_l = [attm, view(WA, 59392, [512], BF16)]
        o32_l = [o32, view(WA, 60416, [256])]
        osq_l = [osq, view(WA, 61440, [256])]
        t1_l = [t1, view(WA, 62464, [256])]
        y16_l = [y16, view(WA, 63488, [256], BF16)]
        ssq4_l = [ssq4, view(WA, 64000, [4])]

        b_hA = [S.buf(f"hA{k}") for k in range(8)]
        b_uTs = [[S.buf(f"uT{k}") for k in range(8)], [S.buf(f"uTb{k}") for k in range(8)]]
        DEEP = False

        def upar(t):
            return (t % 2) if DEEP else 0
        b_xs = [S.buf("xs0"), S.buf("xs1")]
        b_sq = [S.buf("sq0"), S.buf("sq1")]
        b_rstd = S.buf("rstd")
        b_pE, b_pX, b_pY, b_pM, b_pL = (S.buf(n) for n in ("pE", "pX", "pY", "pM", "pL"))
        b_mixp, b_mixg = S.buf("mixp"), S.buf("mixg")
        b_gg, b_S32, b_Sbf, b_stmp, b_ssq4 = (S.buf(n) for n in ("gg", "S32", "Sbf", "stmp", "ssq4"))
        b_qst, b_kst, b_vst = S.buf("qst"), S.buf("kst"), S.buf("vst")
        b_gq, b_gk, b_la, b_Bn, b_eq, b_ek = (S.buf(n) for n in ("gq", "gk", "la", "Bn", "eq", "ek"))
        b_qd, b_ki, b_Qm, b_kitok, b_gv, b_R, b_Eb = (S.buf(n) for n in ("qd", "ki", "Qm", "kitok", "gv", "R", "Eb"))
        b_attm, b_o32, b_osq, b_t1, b_y16 = (S.buf(n) for n in ("attm", "o32", "osq", "t1", "y16"))
        b_attm_l = [b_attm, S.buf("attm2")]
        b_o32_l = [b_o32, S.buf("o32b")]
        b_osq_l = [b_osq, S.buf("osqb")]
        b_t1_l = [b_t1, S.buf("t1b")]
        b_y16_l = [b_y16, S.buf("y16b")]
        b_ssq4_l = [b_ssq4, S.buf("ssq4b")]
        b_hst = S.buf("hst")

        S.op("dve", lambda e: e.memset(qst, 0.0), writes=[b_qst])
        S.op("dve", lambda e: e.memset(pE[:, :, 0:16], 0.0), writes=[b_pE])
        S.op("dve", lambda e: e.memset(S32, 0.0), writes=[b_S32])
        S.op("dve", lambda e: e.memset(Sbf, 0.0), writes=[b_Sbf])
        projbanks = [3, 4]
        pbi = [0]

        def nextbank():
            b = projbanks[pbi[0] % len(projbanks)]
            pbi[0] += 1
            return b

        def load_tile(t):
            if l == 0:
                return
            for kc in range(8):
                S.dma("sp", lambda e, kc=kc: e.dma_start(out=hA[:, kc, :], in_=hT_d[kc * 128:(kc + 1) * 128, t * 512:(t + 1) * 512]),
                      writes=[b_hA[kc]], sembuf=b_hA[kc])

        if l > 0:
            load_tile(0)
        xs4 = view(WA, 43008, [4, 1024])
        b_xs4 = [S.buf(f"xs4_{i}") for i in range(4)]

        def load_x(t):
            for s in range(4):
                S.dma("sp", lambda e, s=s, t=t: e.dma_start(out=xs4[:, s, :], in_=x_d[t * 512 + s * 128:t * 512 + (s + 1) * 128, :]),
                      writes=[b_xs4[s]], sembuf=b_xs4[s])

        cur = [0]

        def proj_fm(col0, M):
            bank = nextbank()
            uT = uTs[cur[0]]
            mm(PS[bank][0:M, :], [(WinV[:, kc, col0:col0 + M], uT[:, kc, :]) for kc in range(8)],
               b_uTs[cur[0]] + [b_win], [PSB[bank]])
            return bank

        def proj_tm(col0, s):
            bank = nextbank()
            uT = uTs[cur[0]]
            mm(PS[bank][:, :], [(uT[:, kc, s * 128:(s + 1) * 128], WinV[:, kc, col0:col0 + 512]) for kc in range(8)],
               b_uTs[cur[0]] + [b_win], [PSB[bank]])
            return bank

        def norm(t):
            c0 = t * 512
            uT = uTs[upar(t)]
            b_uT = b_uTs[upar(t)]
            if l == 0:
                for s in range(4):
                    for half in range(2):
                        bank = half
                        fns = []
                        for kk in range(4):
                            kc = half * 4 + kk
                            fns.append(lambda e, kc=kc, kk=kk, s=s, bank=bank: e.transpose(
                                PS[bank][:, kk * 128:(kk + 1) * 128], xs4[:, s, kc * 128:(kc + 1) * 128], ident32))
                        S.group("pe", fns, [b_xs4[s], b_c32], [PSB[bank]])
                        outv = hA[:, half * 4:(half + 1) * 4, s * 128:(s + 1) * 128]
                        inv = PS[bank][:, :].rearrange("p (a b) -> p a b", a=4)
                        wr = [b_hA[half * 4 + kk] for kk in range(4)]
                        cp(outv, inv, [PSB[bank]], wr, eng="act" if half == 0 else "dve")
                    yield
                if t + 1 < NT:
                    load_x(t + 1)
                S.dma("sp", [lambda e, kc=kc, c0=c0: e.dma_start(out=hT_d[kc * 128:(kc + 1) * 128, c0:c0 + 512], in_=hA[:, kc, :])
                             for kc in range(8)], reads=b_hA, sembuf=b_hst)
            for kc in range(8):
                act(sq[:, kc % 2, :], hA[:, kc, :], AF.Square, [b_hA[kc]], [b_sq[kc % 2]])
                S.group("pe", [lambda e, kc=kc: e.matmul(PS[2][:, :], onesD[:, :], sq[:, kc % 2, :], start=(kc == 0), stop=(kc == 7))],
                        [b_sq[kc % 2], b_ones], [PSB[2]])
                if kc % 2 == 1:
                    yield
            act(rstd, PS[2][:, :], AF.Ln, [PSB[2]], [b_rstd], bias=EPS)
            act(rstd, rstd, AF.Exp, [b_rstd], [b_rstd], scale=-0.5)
            for kc in range(8):
                stt(uT[:, kc, :], hA[:, kc, :], g1[:, kc:kc + 1], rstd, ALU.mult, ALU.mult,
                    [b_hA[kc], b_rstd, b_c32], [b_uT[kc]])
            if l > 0 and t + 1 < NT:
                load_tile(t + 1)
            yield

        def projs(t):
            c0 = t * 512
            for j in range(2):
                cur[0] = upar(t)
                bk = proj_fm(j * 128, 128)
                cp(pE[:, j, 16:528], PS[bk][:, :], [PSB[bk]], [b_pE], eng="act")
                yield
            for h in range(4):
                cur[0] = upar(t)
                bk = proj_fm(256 + h * 128, 128)
                act(qst[0:64, 2 * h, :], PS[bk][0:64, :], AF.Copy, [PSB[bk]], [b_qst], scale=0.125)
                act(qst[64:128, 2 * h + 1, :], PS[bk][64:128, :], AF.Copy, [PSB[bk]], [b_qst], scale=0.125)
                yield
            S.dma("sp", lambda e, c0=c0: e.dma_start(out=qT_d[:, :, c0:c0 + 512].rearrange("a p s -> p a s"), in_=qst),
                  reads=[b_qst], sembuf=b_qst)
            PM = "dve"
            tt(pX[:, :, 1:528], pE[:, :, 1:528], pE[:, :, 0:527], ALU.add, [b_pE], [b_pX], eng=PM)
            tt(pY[64:128, 0, 3:528], pX[64:128, 0, 3:528], pX[64:128, 0, 1:526], ALU.add, [b_pX], [b_pY], eng=PM)
            tt(pY[:, 1, 3:528], pX[:, 1, 3:528], pX[:, 1, 1:526], ALU.add, [b_pX], [b_pY], eng=PM)
            tt(pX[:, 1, 7:528], pY[:, 1, 7:528], pY[:, 1, 3:524], ALU.add, [b_pY], [b_pX], eng=PM)
            tt(pY[64:128, 1, 15:528], pX[64:128, 1, 15:528], pX[64:128, 1, 7:520], ALU.add, [b_pX], [b_pY], eng=PM)
            for h in range(4):
                cur[0] = upar(t)
                bk = proj_fm(768 + h * 128, 128)
                cp(kst[:, h, :], PS[bk][:, :], [PSB[bk]], [b_kst], eng="act")
                yield
            S.dma("sp", lambda e, c0=c0: e.dma_start(out=kT_d[:, :, c0:c0 + 512].rearrange("a p s -> p a s"), in_=kst),
                  reads=[b_kst], sembuf=b_kst)
            ts(pM[0:64, 0, :], pX[0:64, 0, 16:528], 0.5, None, ALU.mult, None, [b_pX], [b_pM], eng=PM)
            ts(pM[64:128, 0, :], pY[64:128, 0, 16:528], 0.25, None, ALU.mult, None, [b_pY], [b_pM], eng=PM)
            ts(pM[0:64, 1, :], pX[0:64, 1, 16:528], 0.125, None, ALU.mult, None, [b_pX], [b_pM], eng=PM)
            ts(pM[64:128, 1, :], pY[64:128, 1, 16:528], 0.0625, None, ALU.mult, None, [b_pY], [b_pM], eng=PM)
            if t == 0:
                corr = c32[:, C_CORR:C_CORR + 32].rearrange("p (a b) -> p a b", a=2)
                tt(pM[:, :, 0:16], pM[:, :, 0:16], corr, ALU.mult, [b_pM, b_c32], [b_pM], eng=PM)
            tt(pL, pM, pE[:, :, 16:528], ALU.subtract, [b_pM, b_pE], [b_pL], eng=PM)
            cp(pE[:, :, 0:16], pE[:, :, 512:528], [b_pE], [b_pE], eng=PM)
            for s in range(4):
                cur[0] = upar(t)
                bk = proj_tm(1280, s)
                cp(vst[:, s, :], PS[bk][:, :], [PSB[bk]], [b_vst], eng="act" if s % 2 == 0 else "dve")
                yield
            S.dma("sp", [lambda e, s=s, t=t: e.dma_start(out=v4_d[:, :, 4 * t + s, :].rearrange("h p d -> p h d"),
                                                    in_=vst[:, s, :].rearrange("p (h d) -> p h d", h=4))
                         for s in range(4)], reads=[b_vst], sembuf=b_vst)
            for j in range(2):
                bk = nextbank()
                mm(PS[bk][:, :], [(poolW[:, l * 256 + j * 128:l * 256 + (j + 1) * 128], pL[:, j, :])], [b_pL, b_poolW], [PSB[bk]])
                ts(mixp[:, j, :], PS[bk][:, :], c32[:, C_PSC + 2 * l + j:C_PSC + 2 * l + j + 1], None, ALU.mult, None,
                   [PSB[bk], b_c32], [b_mixp])
            S.dma("sp", lambda e, c0=c0: e.dma_start(out=mixT_d[0:256, c0:c0 + 512].rearrange("(j p) s -> p j s", p=128), in_=mixp),
                  reads=[b_mixp], sembuf=b_mixp)
            yield

        def stage1b(t):
            cur[0] = upar(t)
            bk = proj_fm(1792, 128)
            cp(gq32, PS[bk][:, :], [PSB[bk]], [b_gq], eng="act")
            bk = proj_fm(1920, 128)
            cp(gk32, PS[bk][:, :], [PSB[bk]], [b_gk], eng="dve")
            bk = proj_fm(2448, 128)
            cp(gg32, PS[bk][:, :], [PSB[bk]], [b_gg], eng="act")
            for s in range(4):
                bk = proj_tm(2048, s)
                cp(gv16[:, s, :], PS[bk][:, 0:256], [PSB[bk]], [b_gv], eng="dve")
                act(Eb[:, s, :], PS[bk][:, 256:512], AF.Exp, [PSB[bk]], [b_Eb], scale=-1.0)
                cp(R[:, s, :], PS[bk][:, 256:512], [PSB[bk]], [b_R], eng="act")

        def gla(t):
            c0 = t * 512
            mm(PS[5][:, :], [(wg2[:, l * 128:(l + 1) * 128], gg32)], [b_wg2, b_gg], [PSB[5]])
            act(la, PS[5][:, :], AF.Exp, [PSB[5], b_misc], [b_la], scale=-1.0, bias=misc[:, M_NBG + l:M_NBG + l + 1])
            act(la, la, AF.Ln, [b_la], [b_la], bias=1.0)
            yield
            for s in range(4):
                S.op("dve", lambda e, s=s: e.tensor_tensor_scan(Bn[:, s * 128:(s + 1) * 128], ones32[:, :], la[:, s * 128:(s + 1) * 128],
                                                                0.0, ALU.mult, ALU.add), [b_la, b_ones], [b_Bn])
            act(eq, Bn, AF.Exp, [b_Bn], [b_eq], scale=-1.0 / 16.0)
            act(ek, Bn, AF.Exp, [b_Bn], [b_ek], scale=1.0 / 16.0)
            stt(qd16, gq32, 32 ** -0.5, eq, ALU.mult, ALU.mult, [b_gq, b_eq], [b_qd])
            tt(ki16, gk32, ek, ALU.mult, [b_gk, b_ek], [b_ki])
            for h in range(4):
                ts(Qm[:, :, h, :], qd16.rearrange("p (a b) -> p a b", a=4), c32[:, C_HM + h:C_HM + h + 1], None, ALU.mult, None,
                   [b_qd, b_c32], [b_Qm])
            yield
            act(Eb, Eb, AF.Ln, [b_Eb], [b_Eb], bias=1.0)
            act(Eb, Eb, AF.Exp, [b_Eb], [b_Eb], scale=-1.0)
            tt(R, R, Eb, ALU.mult, [b_R, b_Eb], [b_R])
            ggl = c32[:, C_GGLA + l * 256:C_GGLA + (l + 1) * 256]
            tt(R, R, ggl.unsqueeze(1).to_broadcast([128, 4, 256]), ALU.mult, [b_R, b_c32], [b_R])
            ps7b = PS[7][:, 0:256].bitcast(BF16)
            S.group("pe", [lambda e, s=s: e.transpose(ps7b[:, s * 128:(s + 1) * 128], ki16[:, s * 128:(s + 1) * 128], identb)
                           for s in range(4)], [b_ki, b_c16], [PSB[7]])
            cp(kitok, ps7b.rearrange("p (a b) -> p a b", a=4), [PSB[7]], [b_kitok], eng="act")
            yield
            ps1b = PS[1][:, 0:128].bitcast(BF16)
            for s in range(4):
                pz = s % 2
                attm, o32, osq, t1, y16, ssq4 = attm_l[pz], o32_l[pz], osq_l[pz], t1_l[pz], y16_l[pz], ssq4_l[pz]
                b_attm, b_o32, b_osq, b_t1, b_y16, b_ssq4 = (b_attm_l[pz], b_o32_l[pz], b_osq_l[pz], b_t1_l[pz], b_y16_l[pz],
                                                             b_ssq4_l[pz])
                mm(PS[5][:, :], [(ki16[:, s * 128:(s + 1) * 128], Qm[:, s, :, :])], [b_ki, b_Qm], [PSB[5]])
                mm(PS[7][:, 256:512], [(kitok[:, s, :], gv16[:, s, :])], [b_kitok, b_gv], [PSB[7]])
                tt(attm, PS[5][:, :], c16[:, K_GM:K_GM + 512], ALU.mult, [PSB[5], b_c16], [b_attm])
                tt(stmp, S32, PS[7][:, 256:512], ALU.add, [b_S32, PSB[7]], [b_stmp])
                yield
                fns = [lambda e, s=s: e.matmul(PS[6][:, 0:256], qd16[:, s * 128:(s + 1) * 128], Sbf, start=True, stop=False)]
                for h in range(4):
                    fns.append(lambda e, s=s, h=h, attm=attm: e.matmul(PS[6][:, h * 64:(h + 1) * 64], attm[:, h * 128:(h + 1) * 128],
                                                                       gv16[:, s, h * 64:(h + 1) * 64], start=False, stop=(h == 3)))
                S.group("pe", fns, [b_qd, b_Sbf, b_attm, b_gv], [PSB[6]])
                cp(o32, PS[6][:, 0:256], [PSB[6]], [b_o32], eng="act")
                stt(S32, stmp, eq[:, s * 128 + 127:s * 128 + 128], c32[:, C_BD:C_BD + 256], ALU.mult, ALU.mult,
                    [b_stmp, b_eq, b_c32], [b_S32])
                cp(Sbf, S32, [b_S32], [b_Sbf], eng="act")
                yield
                tt(osq, o32, o32, ALU.mult, [b_o32], [b_osq])
                S.op("dve", lambda e, osq=osq, ssq4=ssq4: e.tensor_reduce(ssq4, osq.rearrange("p (a b) -> p a b", a=4), AX.X, ALU.add),
                     [b_osq], [b_ssq4])
                act(ssq4, ssq4, AF.Ln, [b_ssq4], [b_ssq4], bias=EPS, scale=1.0 / 64.0)
                act(ssq4, ssq4, AF.Exp, [b_ssq4], [b_ssq4], scale=-0.5)
                tt(t1.rearrange("p (a b) -> p a b", a=4), o32.rearrange("p (a b) -> p a b", a=4),
                   ssq4.unsqueeze(2).to_broadcast([128, 4, 64]), ALU.mult, [b_o32, b_ssq4], [b_t1])
                tt(y16, t1, R[:, s, :], ALU.mult, [b_t1, b_R], [b_y16])
                yield
                S.group("pe", [lambda e, c=c, y16=y16: e.transpose(ps1b[:, c * 128:(c + 1) * 128], y16[:, c * 128:(c + 1) * 128], identb)
                               for c in range(2)], [b_y16, b_c16], [PSB[1]])
                cp(mixg[:, :, s * 128:(s + 1) * 128], ps1b.rearrange("p (a b) -> p a b", a=2), [PSB[1]], [b_mixg], eng="act")
                yield
            S.dma("sp", lambda e, c0=c0: e.dma_start(out=mixT_d[768:1024, c0:c0 + 512].rearrange("(j p) s -> p j s", p=128), in_=mixg),
                  reads=[b_mixg], sembuf=b_mixg)

        def round_robin(gens):
            gens = list(gens)
            while gens:
                for g in list(gens):
                    try:
                        next(g)
                    except StopIteration:
                        gens.remove(g)

        def chain(*gs):
            for g in gs:
                yield from g

        if l == 0:
            load_x(0)
        if DEEP:
            for _ in norm(0):
                pass
        for t in range(NT + 1):
            gens = []
            if t < NT:
                gens.append(projs(t) if DEEP else chain(norm(t), projs(t)))
            if t >= 1:
                gens.append(gla(t - 1))
            if DEEP and t + 1 < NT:
                gens.append(norm(t + 1))
            round_robin(gens)
            if t < NT:
                stage1b(t)

    def phase_B(l):
        kbytes = S_LEN * 2
        KT = [view(WA, 0, [S_LEN], BF16), view(WA, kbytes, [S_LEN], BF16)]
        VV = [view(WA, 2 * kbytes, [NKT, 128], BF16), view(WA, 3 * kbytes, [NKT, 128], BF16)]
        QT = [view(SCR, 2048 * i, [2, 512], BF16) for i in range(3)]
        Pt = [view(SCR, 6144 + 2048 * i, [1024], BF16) for i in range(4)]
        Psum = [view(SCR, 14336 + 2048 * i, [1024], BF16) for i in range(2)]
        Ptmp = view(SCR, 18432, [1024], BF16)
        r1 = view(SCR, 20480, [512])
        r2 = view(SCR, 22528, [512])
        A1 = view(SCR, 24576, [512])
        A2 = view(SCR, 26624, [512])
        ot = view(SCR, 28672, [512])
        osq = view(SCR, 30720, [512], BF16)
        rs = view(SCR, 31744, [512])
        yt = [view(SCR, 33792, [512], BF16), view(SCR, 34816, [512], BF16)]
        b_KT = [S.buf("KT0"), S.buf("KT1")]
        b_VV = [S.buf("VV0"), S.buf("VV1")]
        b_QT = [S.buf(f"QT{i}") for i in range(3)]
        b_P = [S.buf(f"P{i}") for i in range(4)]
        b_Psum = [S.buf("Psum0"), S.buf("Psum1")]
        b_Ptmp = S.buf("Ptmp")
        b_r1, b_r2, b_A1, b_A2, b_ot, b_osq, b_rs = (S.buf(n) for n in ("r1", "r2", "A1", "A2", "ot", "osq", "rs"))
        b_yt = [S.buf("yt0"), S.buf("yt1")]
        nlam = misc[:, M_NLAM + l:M_NLAM + l + 1]

        def load_head(h):
            S.dma("sp", lambda e: e.dma_start(out=KT[h % 2], in_=kT_d[h]), writes=[b_KT[h % 2]], sembuf=b_KT[h % 2])
            S.dma("sp", lambda e: e.dma_start(out=VV[h % 2], in_=v4_d[h]), writes=[b_VV[h % 2]], sembuf=b_VV[h % 2])

        jobs = [(h, qt) for h in range(4) for qt in range(NT)]

        def load_q(ji):
            h, qt = jobs[ji]
            S.dma("sp", lambda e: e.dma_start(out=QT[ji % 3], in_=qT_d[2 * h:2 * h + 2, :, qt * 512:(qt + 1) * 512].rearrange("m p s -> p m s")),
                  writes=[b_QT[ji % 3]], sembuf=b_QT[ji % 3])

        PEA = True
        njobs = len(jobs)
        steps = [(ji, kt) for ji, (h, qt) in enumerate(jobs) for kt in range(4 * qt + 4)]
        NS = len(steps)
        later = {}

        def defer(i, fn):
            later.setdefault(i, []).append(fn)

        def qk(i):
            ji, kt = steps[i]
            h, qt = jobs[ji]
            if kt == 2 and qt == 0 and h + 1 < 4:
                load_head(h + 1)
            if kt == 0 and ji + 2 < njobs:
                load_q(ji + 2)
            K_, Q_ = KT[h % 2], QT[ji % 3]
            b0 = 2 * (kt % 2)
            S.group("pe", [lambda e, m=m: e.matmul(PS[b0 + m][:, :], K_[64 * m:64 * m + 64, kt * 128:(kt + 1) * 128],
                                                   Q_[64 * m:64 * m + 64, m, :], start=True, stop=True) for m in range(2)],
                    [b_KT[h % 2], b_QT[ji % 3]], [PSB[b0], PSB[b0 + 1]], attach=PEA)

        def expo(i):
            ji, kt = steps[i]
            h, qt = jobs[ji]
            nk = 4 * qt + 4
            b0 = 2 * (kt % 2)
            P_ = Pt[kt % 4]
            bP = b_P[kt % 4]
            act(P_, PSALL[:, b0 * 512:(b0 + 2) * 512], AF.Exp, [PSB[b0], PSB[b0 + 1]], [bP])
            if kt >= 4 * qt:
                r = kt - 4 * qt
                msk = c16[:, K_AM + r * 512:K_AM + (r + 1) * 512].unsqueeze(1).to_broadcast([128, 2, 512])
                P3 = P_.rearrange("p (a b) -> p a b", a=2)
                tt(P3, P3, msk, ALU.mult, [bP, b_c16], [bP])
            g = kt // 4
            if kt % 4 == 1:
                tt(Psum[g % 2], Pt[(kt - 1) % 4], P_, ALU.add, [b_P[(kt - 1) % 4], bP], [b_Psum[g % 2]])
            if kt % 4 == 3 and kt != nk - 1:
                tt(Ptmp, Pt[(kt - 1) % 4], P_, ALU.add, [b_P[(kt - 1) % 4], bP], [b_Ptmp])
                tt(Psum[g % 2], Psum[g % 2], Ptmp, ALU.add, [b_Ptmp, b_Psum[g % 2]], [b_Psum[g % 2]])

        def den_mm(rhs_ap, rbufs, first, last):
            for m in range(2):
                S.group("pe", [lambda e, m=m, first=first, last=last: e.matmul(
                    PS[6 + m][:, :], ones16[:, :], rhs_ap[:, m * 512:(m + 1) * 512], start=first, stop=last)],
                    [b_ones] + rbufs, [PSB[6 + m]], attach=PEA)

        def av(i):
            ji, kt = steps[i]
            h, qt = jobs[ji]
            nk = 4 * qt + 4
            V_ = VV[h % 2]
            P_ = Pt[kt % 4]
            for m in range(2):
                S.group("pe", [lambda e, m=m: e.matmul(
                    PS[4 + m][:, :], V_[:, kt, :], P_[:, m * 512:(m + 1) * 512], start=(kt == 0), stop=(kt == nk - 1))],
                    [b_VV[h % 2], b_P[kt % 4]], [PSB[4 + m]], attach=PEA)
            if kt % 4 == 1 and kt >= 5:
                g = (kt - 5) // 4
                den_mm(Psum[g % 2], [b_Psum[g % 2]], g == 0, False)
            if kt == nk - 1:
                g = nk // 4 - 1
                den_mm(Psum[g % 2], [b_Psum[g % 2]], g == 0, False)
                den_mm(Pt[(kt - 1) % 4], [b_P[(kt - 1) % 4]], False, False)
                den_mm(P_, [b_P[kt % 4]], False, True)
                cp(A1, PS[4][:, :], [PSB[4]], [b_A1], eng="dve")
                act(r1, PS[6][:, :], AF.Ln, [PSB[6]], [b_r1])
                cp(A2, PS[5][:, :], [PSB[5]], [b_A2], eng="dve")
                act(r2, PS[7][:, :], AF.Ln, [PSB[7]], [b_r2])

                def e_a():
                    act(r1, r1, AF.Exp, [b_r1], [b_r1], scale=-1.0)
                    act(r2, r2, AF.Exp, [b_r2], [b_r2], scale=-1.0)

                def e_b():
                    tt(A1, A1, r1, ALU.mult, [b_A1, b_r1], [b_A1])
                    tt(A2, A2, r2, ALU.mult, [b_A2, b_r2], [b_A2])
                    stt(ot, A2, nlam, A1, ALU.mult, ALU.add, [b_A2, b_A1, b_misc], [b_ot])

                def e_c():
                    act(osq, ot, AF.Square, [b_ot], [b_osq])

                def e_d():
                    mm(PS[0][:, :], [(onesV[:, :], osq)], [b_osq, b_ones], [PSB[0]])
                    act(rs, PS[0][:, :], AF.Ln, [PSB[0]], [b_rs], bias=EPS)
                    act(rs, rs, AF.Exp, [b_rs], [b_rs], scale=-0.5)

                def e_e(ji=ji, h=h, qt=qt):
                    y_ = yt[ji % 2]
                    stt(y_, ot, misc[:, M_GD + 4 * l + h:M_GD + 4 * l + h + 1], rs, ALU.mult, ALU.mult, [b_ot, b_rs, b_misc], [b_yt[ji % 2]])
                    S.dma("sp", lambda e: e.dma_start(out=mixT_d[256 + h * 128:256 + (h + 1) * 128, qt * 512:(qt + 1) * 512], in_=y_),
                          reads=[b_yt[ji % 2]], sembuf=b_yt[ji % 2])
                for d_, f_ in enumerate((e_a, e_b, e_c, e_d, e_e)):
                    defer(i + 2 + d_, f_)

        load_head(0)
        load_q(0)
        if njobs > 1:
            load_q(1)
        qk(0)
        for i in range(NS):
            if i + 1 < NS:
                qk(i + 1)
            if i >= 1:
                av(i - 1)
            expo(i)
            for f_ in later.pop(i, []):
                f_()
        av(NS - 1)
        for k_ in sorted(later):
            for f_ in later[k_]:
                f_()

    def phase_C(l):
        g2 = c32[:, C_G2 + l * 8:C_G2 + (l + 1) * 8]
        hA = [view(SCR, 0, [8, 512]), view(SCR, 16384, [8, 512])]
        mx = [view(SCR, 32768, [8, 512], BF16), view(SCR, 40960, [8, 512], BF16)]
        sq = view(WB, 16384, [8, 512], BF16)
        rstd = view(SCR, 51200, [512])
        zT = view(SCR, 53248, [8, 512], BF16)
        b_hA = [S.buf("ChA0"), S.buf("ChA1")]
        b_mx = [S.buf("Cmx0"), S.buf("Cmx1")]
        b_sq = [S.buf(f"Csq{c}") for c in range(8)]
        b_rstd, b_zT = S.buf("Crstd"), S.buf("CzT")

        def load(t):
            i = t % 2
            S.dma("sp", lambda e: e.dma_start(out=hA[i], in_=hT_d[:, t * 512:(t + 1) * 512].rearrange("(k p) s -> p k s", p=128)),
                  writes=[b_hA[i]], sembuf=b_hA[i])
            S.dma("sp", lambda e: e.dma_start(out=mx[i], in_=mixT_d[:, t * 512:(t + 1) * 512].rearrange("(k p) s -> p k s", p=128)),
                  writes=[b_mx[i]], sembuf=b_mx[i])

        load(0)
        for t in range(NT):
            i = t % 2
            if t + 1 < NT:
                load(t + 1)
            for c in range(8):
                bank = c % 4
                mm(PS[bank][:, :], [(WoutV[:, k, c * 128:(c + 1) * 128], mx[i][:, k, :]) for k in range(8)],
                   [b_wout, b_mx[i]], [PSB[bank]])
                tt(hA[i][:, c, :], hA[i][:, c, :], PS[bank][:, :], ALU.add, [PSB[bank], b_hA[i]], [b_hA[i]])
                act(sq[:, c, :], hA[i][:, c, :], AF.Square, [b_hA[i]], [b_sq[c]])
            for c in range(8):
                S.group("pe", [lambda e, c=c: e.matmul(PS[4][:, :], onesD[:, :], sq[:, c, :], start=(c == 0), stop=(c == 7))],
                        [b_sq[c], b_ones], [PSB[4]])
            act(rstd, PS[4][:, :], AF.Ln, [PSB[4]], [b_rstd], bias=EPS)
            act(rstd, rstd, AF.Exp, [b_rstd], [b_rstd], scale=-0.5)
            for c in range(8):
                stt(zT[:, c, :], hA[i][:, c, :], g2[:, c:c + 1], rstd, ALU.mult, ALU.mult, [b_hA[i], b_rstd, b_c32], [b_zT])
            S.dma("sp", lambda e, i=i, t=t: e.dma_start(out=hT_d[:, t * 512:(t + 1) * 512].rearrange("(k p) s -> p k s", p=128), in_=hA[i]),
                  reads=[b_hA[i]], sembuf=b_hA[i])
            S.dma("sp", lambda e, t=t: e.dma_start(out=zT_d[:, t * 512:(t + 1) * 512].rearrange("(k p) s -> p k s", p=128), in_=zT),
                  reads=[b_zT], sembuf=b_zT)

    def phase_D(l, hf, final):
        W1 = W1V[hf]
        W2 = W2V[hf]
        hA = [view(SCR, 0, [8, 512]), view(SCR, 16384, [8, 512])]
        zT = view(SCR, 32768, [8, 512], BF16)
        hid = view(SCR, 40960, [16, 512], BF16)
        rt = [view(SCR, 57344, [512]), view(SCR, 59392, [512])]
        sq = view(SCR, 61440, [2, 512], BF16)
        rstd = view(SCR, 63488, [512])
        ost = [view(WA, 0, [1024]), view(WA, 4096, [1024])]
        b_hA = [S.buf("DhA0"), S.buf("DhA1")]
        b_zT = S.buf("DzT")
        b_hid = [S.buf(f"hid{f}") for f in range(16)]
        b_rt = [S.buf("rt0"), S.buf("rt1")]
        b_sq = [S.buf("Dsq0"), S.buf("Dsq1")]
        b_rstd = S.buf("Drstd")
        b_ost = [S.buf("ost0"), S.buf("ost1")]
        gf = c32[:, C_GF:C_GF + 8]

        def load_h(t):
            i = t % 2
            S.dma("sp", lambda e: e.dma_start(out=hA[i], in_=hT_d[:, t * 512:(t + 1) * 512].rearrange("(k p) s -> p k s", p=128)),
                  writes=[b_hA[i]], sembuf=b_hA[i])

        def load_z(t):
            S.dma("sp", lambda e: e.dma_start(out=zT, in_=zT_d[:, t * 512:(t + 1) * 512].rearrange("(k p) s -> p k s", p=128)),
                  writes=[b_zT], sembuf=b_zT)

        load_h(0)
        load_z(0)
        oi = 0
        for t in range(NT):
            i = t % 2
            if t + 1 < NT:
                load_h(t + 1)
            for f in range(16):
                bank = f % 4
                mm(PS[bank][:, :], [(W1[:, k, f * 128:(f + 1) * 128], zT[:, k, :]) for k in range(8)], [b_w1[hf], b_zT], [PSB[bank]])
                r_ = rt[f % 2]
                act(r_, PS[bank][:, :], AF.Relu, [PSB[bank]], [b_rt[f % 2]])
                tt(hid[:, f, :], r_, PS[bank][:, :], ALU.mult, [b_rt[f % 2], PSB[bank]], [b_hid[f]])
            if t + 1 < NT:
                load_z(t + 1)
            for c in range(8):
                bank = 4 + c % 4
                mm(PS[bank][:, :], [(W2[:, f, c * 128:(c + 1) * 128], hid[:, f, :]) for f in range(16)], [b_w2[hf]] + b_hid, [PSB[bank]])
                tt(hA[i][:, c, :], hA[i][:, c, :], PS[bank][:, :], ALU.add, [PSB[bank], b_hA[i]], [b_hA[i]])
            if not final:
                S.dma("sp", lambda e, i=i, t=t: e.dma_start(out=hT_d[:, t * 512:(t + 1) * 512].rearrange("(k p) s -> p k s", p=128), in_=hA[i]),
                      reads=[b_hA[i]], sembuf=b_hA[i])
            else:
                for c in range(8):
                    act(sq[:, c % 2, :], hA[i][:, c, :], AF.Square, [b_hA[i]], [b_sq[c % 2]])
                    S.group("pe", [lambda e, c=c: e.matmul(PS[0][:, :], onesD[:, :], sq[:, c % 2, :], start=(c == 0), stop=(c == 7))],
                            [b_sq[c % 2], b_ones], [PSB[0]])
                act(rstd, PS[0][:, :], AF.Ln, [PSB[0]], [b_rstd], bias=EPS)
                act(rstd, rstd, AF.Exp, [b_rstd], [b_rstd], scale=-0.5)
                for c in range(8):
                    stt(hA[i][:, c, :], hA[i][:, c, :], gf[:, c:c + 1], rstd, ALU.mult, ALU.mult, [b_hA[i], b_rstd, b_c32], [b_hA[i]])
                for s in range(4):
                    o_ = ost[oi % 2]
                    bo = b_ost[oi % 2]
                    for half in range(2):
                        bank = 1 + half
                        S.group("pe", [lambda e, kk=kk, half=half, bank=bank, s=s, i=i: e.transpose(
                            PS[bank][:, kk * 128:(kk + 1) * 128], hA[i][:, half * 4 + kk, s * 128:(s + 1) * 128], ident32) for kk in range(4)],
                            [b_hA[i], b_c32], [PSB[bank]])
                        cp(o_[:, half * 512:(half + 1) * 512], PS[bank][:, :], [PSB[bank]], [bo], eng="act" if half == 0 else "dve")
                    S.dma("sp", lambda e, o_=o_, t=t, s=s: e.dma_start(out=out_d[t * 512 + s * 128:t * 512 + (s + 1) * 128, :], in_=o_),
                          reads=[bo], sembuf=bo)
                    oi += 1

    def done(tag):
        return stop_after is not None and stop_after == tag

    S.barrier()
    finished = False
    for l in range(n_layers):
        if l > 0:
            load_wout(l)
        phase_A(l)
        S.barrier()
        if done(f"A{l}"):
            break
        phase_B(l)
        S.barrier()
        if done(f"B{l}"):
            break
        load_w12(l, 0)
        phase_C(l)
        S.barrier()
        if done(f"C{l}"):
            break
        load_w12(l, 1)
        phase_D(l, 0, False)
        S.barrier()
        if l + 1 < n_layers:
            load_win(l + 1)
        phase_D(l, 1, l == n_layers - 1)
        S.barrier()
    S.finish()
    return nc, S


def _host_inputs(inp, core_x):
    c32, c16 = _const_tables()
    c32 = _fill_params(c32, inp)
    wg2 = np.ascontiguousarray(np.asarray(inp["gla_w_gate2"], np.float32).transpose(1, 0, 2).reshape(16, L * 128))
    return {
        "x": np.ascontiguousarray(core_x, dtype=np.float32),
        "w_in": np.asarray(inp["w_in"], np.float32),
        "w_out": np.asarray(inp["w_out"], np.float32),
        "w_mlp1": np.asarray(inp["w_mlp1"], np.float32),
        "w_mlp2": np.asarray(inp["w_mlp2"], np.float32),
        "wg2": wg2,
        "pool_w": np.asarray(inp["pool_w"], np.float32),
        "c32": c32,
        "c16": c16,
    }


def kernel(**inputs):
    x = np.asarray(inputs["x"], np.float32)
    B, S_LEN, _ = x.shape
    nc, _ = build_nc(S_LEN)
    base = _host_inputs(inputs, x[0])
    in_maps = []
    for b in range(B):
        m = dict(base)
        m["x"] = np.ascontiguousarray(x[b])
        in_maps.append(m)
    res = run_bass_kernel_spmd(nc, in_maps, core_ids=list(range(B)))
    return np.stack([np.asarray(r["out"], np.float32) for r in res.results], axis=0)
```
